# Optimizing a Trainium2 kernel written in Bass

```python
import jax, jax.numpy as jnp
from jax import lax
import numpy as np

D_MODEL = 4096
BATCH = 1
SEQ = 16384
DEPTH = 4

CHUNK = 64
N_MIXERS = 3
ATTN_HEADS = 32
ATTN_HEAD_DIM = D_MODEL // ATTN_HEADS
LEFT_CHUNKS = 8
BAND = (LEFT_CHUNKS + 1) * CHUNK
MAX_REL = 128
CONV_WIDTH = 31
POOL_WINDOWS = (2, 4, 8, 16)
N_POOL_GROUPS = len(POOL_WINDOWS)
POOL_GROUP_WIDTH = D_MODEL // N_POOL_GROUPS
N_MEM = 256
MEM_HEADS = 4
MEM_HEAD_DIM = 128
MEM_INNER = MEM_HEADS * MEM_HEAD_DIM
N_GROUPS = 4
EXPERTS_PER_GROUP = 8
N_EXPERTS = N_GROUPS * EXPERTS_PER_GROUP
TOP_K = 2
D_EXPERT = 384
EXPERT_BLOCK = 128
LN_EPS = 1e-5
DEEPNORM_ALPHA = (2 * DEPTH) ** 0.25
DEEPNORM_BETA = (8 * DEPTH) ** -0.25

kernel_name = 'streaming_hybrid_interleaved_moe'


def layer_norm(x, g, b):
    xf = x.astype(jnp.float32)
    mu = jnp.mean(xf, axis=-1, keepdims=True)
    var = jnp.mean(jnp.square(xf - mu), axis=-1, keepdims=True)
    y = (xf - mu) * lax.rsqrt(var + LN_EPS) * g.astype(jnp.float32) + b.astype(jnp.float32)
    return y.astype(x.dtype)


def chunked_rel_attention(h, w_qkv, w_o, rel_table):
    B, S, D = h.shape
    n_chunks = S // CHUNK
    q, k, v = jnp.split(h @ w_qkv, 3, axis=-1)
    q = q.reshape(B, S, ATTN_HEADS, ATTN_HEAD_DIM) * (ATTN_HEAD_DIM ** -0.5)
    k = k.reshape(B, S, ATTN_HEADS, ATTN_HEAD_DIM)
    v = v.reshape(B, S, ATTN_HEADS, ATTN_HEAD_DIM)
    pad = LEFT_CHUNKS * CHUNK
    k_pad = jnp.pad(k, ((0, 0), (pad, 0), (0, 0), (0, 0)))
    v_pad = jnp.pad(v, ((0, 0), (pad, 0), (0, 0), (0, 0)))
    q_c = q.reshape(B, n_chunks, CHUNK, ATTN_HEADS, ATTN_HEAD_DIM).transpose(1, 0, 2, 3, 4)
    qq = jnp.arange(CHUNK)
    kk = jnp.arange(BAND)
    rel = qq[:, None] + pad - kk[None, :]
    rel_idx = jnp.clip(rel, -MAX_REL, MAX_REL) + MAX_REL
    bias = rel_table[:, rel_idx].astype(jnp.float32)

    def one_chunk(args):
        c, q_blk = args
        start = c * CHUNK
        k_blk = lax.dynamic_slice_in_dim(k_pad, start, BAND, axis=1)
        v_blk = lax.dynamic_slice_in_dim(v_pad, start, BAND, axis=1)
        s = jnp.einsum('bqhd,bkhd->bhqk', q_blk, k_blk).astype(jnp.float32) + bias[None]
        key_pos = start - pad + kk
        s = jnp.where((key_pos >= 0)[None, None, None, :], s, -1e30)
        p = jax.nn.softmax(s, axis=-1).astype(v_blk.dtype)
        return jnp.einsum('bhqk,bkhd->bqhd', p, v_blk)

    o = lax.map(one_chunk, (jnp.arange(n_chunks), q_c))
    o = o.transpose(1, 0, 2, 3, 4).reshape(B, S, D)
    return o @ w_o


def conformer_conv(h, w_in, b_in, w_dw, b_dw, ln_g, ln_b, w_out, b_out):
    D = h.shape[-1]
    a, g = jnp.split(h @ w_in + b_in, 2, axis=-1)
    u = a * jax.nn.sigmoid(g)
    u = lax.conv_general_dilated(u, w_dw[:, None, :], window_strides=(1,), padding=[(CONV_WIDTH - 1, 0)],
                                 dimension_numbers=('NWC', 'WIO', 'NWC'), feature_group_count=D) + b_dw
    u = jax.nn.silu(layer_norm(u, ln_g, ln_b))
    return u @ w_out + b_out


def multiscale_pool(h, w_pool, scale):
    B, S, D = h.shape
    xg = h.reshape(B, S, N_POOL_GROUPS, POOL_GROUP_WIDTH).astype(jnp.float32)
    cs = jnp.pad(jnp.cumsum(xg, axis=1), ((0, 0), (1, 0), (0, 0), (0, 0)))
    t = jnp.arange(S)[:, None]
    win = jnp.array(POOL_WINDOWS, dtype=jnp.int32)[None, :]
    lo = jnp.maximum(t + 1 - win, 0)
    cnt = (t + 1 - lo).astype(jnp.float32)
    grp = jnp.arange(N_POOL_GROUPS)[None, :]
    window_sum = cs[:, 1:] - cs[:, lo, grp]
    pooled = (window_sum / cnt[None, :, :, None] - xg).astype(h.dtype)
    y = jnp.einsum('bsgc,gcd->bsgd', pooled, w_pool).reshape(B, S, D)
    return y * scale


def memory_cross_attention(h, mem, w_q, w_kv, w_o):
    B, S, _ = h.shape
    q = (h @ w_q).reshape(B, S, MEM_HEADS, MEM_HEAD_DIM) * (MEM_HEAD_DIM ** -0.5)
    k, v = jnp.split(mem @ w_kv, 2, axis=-1)
    k = k.reshape(B, -1, MEM_HEADS, MEM_HEAD_DIM)
    v = v.reshape(B, -1, MEM_HEADS, MEM_HEAD_DIM)
    s = jnp.einsum('bshd,bmhd->bhsm', q, k).astype(jnp.float32)
    p = jax.nn.softmax(s, axis=-1).astype(v.dtype)
    o = jnp.einsum('bhsm,bmhd->bshd', p, v).reshape(B, S, MEM_INNER)
    return o @ w_o


def grouped_expert_ffn(hf, expert_ids, gates, w_gate, w_up, w_down):
    T, D = hf.shape
    n_assign = T * TOP_K
    flat_e = expert_ids.reshape(-1)
    flat_tok = jnp.repeat(jnp.arange(T, dtype=jnp.int32), TOP_K)
    flat_gate = gates.reshape(-1)
    order = jnp.argsort(flat_e)
    se = flat_e[order]
    counts = jnp.bincount(flat_e, length=N_EXPERTS)
    padded = (counts + EXPERT_BLOCK - 1) // EXPERT_BLOCK * EXPERT_BLOCK
    starts = jnp.cumsum(counts) - counts
    ends_p = jnp.cumsum(padded)
    pstarts = ends_p - padded
    dest = pstarts[se] + jnp.arange(n_assign, dtype=jnp.int32) - starts[se]
    n_blocks = -(-n_assign // EXPERT_BLOCK) + N_EXPERTS
    buf_len = n_blocks * EXPERT_BLOCK
    tok_buf = jnp.full((buf_len,), T, dtype=jnp.int32).at[dest].set(flat_tok[order])
    gate_buf = jnp.zeros((buf_len,), jnp.float32).at[dest].set(flat_gate[order])
    block_expert = jnp.minimum(jnp.searchsorted(ends_p, jnp.arange(n_blocks) * EXPERT_BLOCK, side='right'),
                               N_EXPERTS - 1)
    h_pad = jnp.concatenate([hf, jnp.zeros((1, D), hf.dtype)], axis=0)

    def run_block(args):
        tok, e = args
        xe = h_pad[tok]
        u = jax.nn.silu(xe @ w_gate[e]) * (xe @ w_up[e])
        return u @ w_down[e]

    yb = lax.map(run_block, (tok_buf.reshape(n_blocks, EXPERT_BLOCK), block_expert)).reshape(buf_len, D)
    y = jnp.zeros((T + 1, D), hf.dtype).at[tok_buf].add(yb * gate_buf[:, None].astype(yb.dtype))
    return y[:T]


def hierarchical_moe(h, w_group, b_group, w_router, b_router, w_gate, w_up, w_down):
    B, S, D = h.shape
    hf = h.reshape(B * S, D)
    g_prob = jax.nn.softmax((hf @ w_group + b_group).astype(jnp.float32), axis=-1)
    g_sel = jnp.argmax(g_prob, axis=-1).astype(jnp.int32)
    g_w = jnp.take_along_axis(g_prob, g_sel[:, None], axis=-1)
    e_logits_all = jnp.einsum('td,gde->tge', hf, w_router) + b_router
    e_logits = jnp.take_along_axis(e_logits_all, g_sel[:, None, None], axis=1)[:, 0].astype(jnp.float32)
    e_prob = jax.nn.softmax(e_logits, axis=-1)
    top_p, top_i = lax.top_k(e_prob, TOP_K)
    gates = top_p / jnp.sum(top_p, axis=-1, keepdims=True) * g_w
    expert_ids = g_sel[:, None] * EXPERTS_PER_GROUP + top_i.astype(jnp.int32)
    y = grouped_expert_ffn(hf, expert_ids, gates, w_gate, w_up, w_down)
    return y.reshape(B, S, D)


def setup_inputs(seed: int = 0) -> dict:
    key = jax.random.key(seed)
    keys = list(jax.random.split(key, 40))

    def nrm(shape, scale):
        return jax.random.normal(keys.pop(), shape, jnp.float32) * scale

    D = D_MODEL
    s = D ** -0.5
    beta = DEEPNORM_BETA
    n_a = len(range(0, DEPTH, N_MIXERS))
    n_b = len(range(1, DEPTH, N_MIXERS))
    n_c = len(range(2, DEPTH, N_MIXERS))
    return {
        'x': nrm((BATCH, SEQ, D), 1.0),
        'mem': nrm((BATCH, N_MEM, D), 1.0),
        'attn_w_qkv': jnp.concatenate([nrm((n_a, D, 2 * D), s), nrm((n_a, D, D), beta * s)], axis=-1),
        'attn_w_o': nrm((n_a, D, D), beta * s),
        'attn_rel_bias': nrm((n_a, ATTN_HEADS, 2 * MAX_REL + 1), 0.5),
        'conv_w_in': nrm((n_b, D, 2 * D), s),
        'conv_b_in': nrm((n_b, 2 * D), 0.02),
        'conv_w_dw': nrm((n_b, CONV_WIDTH, D), CONV_WIDTH ** -0.5),
        'conv_b_dw': nrm((n_b, D), 0.02),
        'conv_ln_g': 1.0 + nrm((n_b, D), 0.02),
        'conv_ln_b': nrm((n_b, D), 0.02),
        'conv_w_out': nrm((n_b, D, D), beta * s),
        'conv_b_out': nrm((n_b, D), 0.02),
        'pool_w': nrm((n_c, N_POOL_GROUPS, POOL_GROUP_WIDTH, POOL_GROUP_WIDTH), beta * POOL_GROUP_WIDTH ** -0.5),
        'pool_scale': 1.0 + nrm((n_c, D), 0.1),
        'mem_w_q': nrm((DEPTH, D, MEM_INNER), s),
        'mem_w_kv': jnp.concatenate([nrm((DEPTH, D, MEM_INNER), s), nrm((DEPTH, D, MEM_INNER), beta * s)], axis=-1),
        'mem_w_o': nrm((DEPTH, MEM_INNER, D), beta * MEM_INNER ** -0.5),
        'moe_w_group': nrm((DEPTH, D, N_GROUPS), s),
        'moe_b_group': nrm((DEPTH, N_GROUPS), 0.01),
        'moe_w_router': nrm((DEPTH, N_GROUPS, D, EXPERTS_PER_GROUP), s),
        'moe_b_router': nrm((DEPTH, N_GROUPS, EXPERTS_PER_GROUP), 0.01),
        'moe_w_gate': nrm((DEPTH, N_EXPERTS, D, D_EXPERT), s),
        'moe_w_up': nrm((DEPTH, N_EXPERTS, D, D_EXPERT), s),
        'moe_w_down': nrm((DEPTH, N_EXPERTS, D_EXPERT, D), beta * D_EXPERT ** -0.5),
        'ln_g': 1.0 + nrm((DEPTH, 3, D), 0.02),
        'ln_b': nrm((DEPTH, 3, D), 0.02),
    }


def reference(x, mem, attn_w_qkv, attn_w_o, attn_rel_bias, conv_w_in, conv_b_in, conv_w_dw, conv_b_dw,
              conv_ln_g, conv_ln_b, conv_w_out, conv_b_out, pool_w, pool_scale, mem_w_q, mem_w_kv, mem_w_o,
              moe_w_group, moe_b_group, moe_w_router, moe_b_router, moe_w_gate, moe_w_up, moe_w_down,
              ln_g, ln_b):
    for i in range(DEPTH):
        kind = i % N_MIXERS
        j = i // N_MIXERS
        if kind == 0:
            f = chunked_rel_attention(x, attn_w_qkv[j], attn_w_o[j], attn_rel_bias[j])
        elif kind == 1:
            f = conformer_conv(x, conv_w_in[j], conv_b_in[j], conv_w_dw[j], conv_b_dw[j], conv_ln_g[j],
                               conv_ln_b[j], conv_w_out[j], conv_b_out[j])
        else:
            f = multiscale_pool(x, pool_w[j], pool_scale[j])
        x = layer_norm(DEEPNORM_ALPHA * x + f, ln_g[i, 0], ln_b[i, 0])
        c = memory_cross_attention(x, mem, mem_w_q[i], mem_w_kv[i], mem_w_o[i])
        x = layer_norm(DEEPNORM_ALPHA * x + c, ln_g[i, 1], ln_b[i, 1])
        m = hierarchical_moe(x, moe_w_group[i], moe_b_group[i], moe_w_router[i], moe_b_router[i],
                             moe_w_gate[i], moe_w_up[i], moe_w_down[i])
        x = layer_norm(DEEPNORM_ALPHA * x + m, ln_g[i, 2], ln_b[i, 2])
    return x
```

```python
import numpy as np
from contextlib import ExitStack
import concourse.bass as bass
import concourse.mybir as mybir
from concourse.bass_utils import run_bass_kernel_spmd

F32 = mybir.dt.float32
BF16 = mybir.dt.bfloat16
I32 = mybir.dt.int32
AF = mybir.ActivationFunctionType
ALU = mybir.AluOpType
AX = mybir.AxisListType

ENGS = ["pe", "act", "dve", "pool", "sp"]
NDMA = 90

D = 4096
KC = 32
ALPHA = 8.0 ** 0.25
EPS = 1e-5
NEG = -30000.0
DEXP = 384
NEXP = 32
CAPB = 3
CAP = CAPB * 128
NSLOT = NEXP * CAP


class Buf:
    __slots__ = ("name", "w", "r", "acc", "dsem")

    def __init__(self, name, acc=False):
        self.name = name
        self.w = {}
        self.r = {}
        self.acc = acc
        self.dsem = None


class Sched:
    def __init__(self, nc, stack):
        self.nc = nc
        self.sems = []
        self.eng = {}
        for name in ENGS:
            h = stack.enter_context(nc.semaphore("s_" + name))
            self.sems.append(h)
            self.eng[name] = dict(sem=len(self.sems) - 1, cnt=0, seen={}, prog=[], pending={})
        self.dma_sems = []
        for i in range(NDMA):
            h = stack.enter_context(nc.semaphore("d%d" % i))
            self.sems.append(h)
            self.dma_sems.append(len(self.sems) - 1)
        self.dma_val = {s: 0 for s in self.dma_sems}
        self.dma_free = list(self.dma_sems)
        self.n_ops = 0

    def dsem_of(self, buf):
        if buf.dsem is None:
            buf.dsem = self.dma_free.pop(0)
        return buf.dsem

    def release_dsem(self, buf):
        if buf.dsem is not None:
            self.dma_free.append(buf.dsem)
            buf.dsem = None

    def barrier(self):
        snap = {}
        for name in ENGS:
            E = self.eng[name]
            if E["cnt"] > 0:
                snap[E["sem"]] = E["cnt"]
        for s, v in self.dma_val.items():
            if v > 0:
                snap[s] = v
        for name in ENGS:
            E = self.eng[name]
            for s, v in snap.items():
                if E["pending"].get(s, 0) < v:
                    E["pending"][s] = v

    def _waits(self, E, own, reads, writes, skip_own):
        waits = dict(E["pending"])
        E["pending"] = {}
        for b in reads:
            for s, v in b.w.items():
                if v > waits.get(s, 0):
                    waits[s] = v
        for b in writes:
            if not b.acc:
                for s, v in b.w.items():
                    if v > waits.get(s, 0):
                        waits[s] = v
            for s, v in b.r.items():
                if v > waits.get(s, 0):
                    waits[s] = v
        need = []
        seen = E["seen"]
        for s, v in waits.items():
            if s == own and skip_own:
                continue
            if seen.get(s, 0) < v:
                seen[s] = v
                need.append((s, v))
        return need

    def op(self, eng, fn, reads=(), writes=()):
        E = self.eng[eng]
        own = E["sem"]
        need = self._waits(E, own, reads, writes, skip_own=(eng == "pe"))
        E["cnt"] += 1
        v = E["cnt"]
        E["prog"].append((need, fn, own, 1))
        for b in reads:
            if b.r.get(own, 0) < v:
                b.r[own] = v
        for b in writes:
            if b.acc:
                b.w[own] = v
            else:
                b.w = {own: v}
                b.r = {}
        self.n_ops += 1

    def dma(self, q, fn, sbuf, reads=(), writes=()):
        E = self.eng[q]
        d = self.dsem_of(sbuf)
        wr = []
        join = set()
        for b in writes:
            if (not b.acc) and len(b.r) == 0 and len(b.w) > 0 and set(b.w.keys()) <= {d}:
                join.add(id(b))
                continue
            wr.append(b)
        need = self._waits(E, None, reads, wr, skip_own=False)
        self.dma_val[d] += 16
        v = self.dma_val[d]
        E["prog"].append((need, fn, d, 16))
        for b in reads:
            if b.r.get(d, 0) < v:
                b.r[d] = v
        for b in writes:
            if b.acc or id(b) in join:
                b.w[d] = v
            else:
                b.w = {d: v}
                b.r = {}
        self.n_ops += 1

    def emit(self, block):
        sems = self.sems
        final = []
        for name in ENGS:
            E = self.eng[name]
            if name != "sp" and E["cnt"] > 0:
                final.append((E["sem"], E["cnt"]))
        for s, v in self.dma_val.items():
            if v > 0:
                final.append((s, v))

        def mk(name):
            E = self.eng[name]

            def body(eng):
                for need, fn, s, inc in E["prog"]:
                    for ws, wv in need:
                        eng.wait_ge(sems[ws], wv)
                    fn(eng).then_inc(sems[s], inc)
                if name == "sp":
                    for ws, wv in final:
                        eng.wait_ge(sems[ws], wv)
            return body

        block.tensor(mk("pe"))
        block.scalar(mk("act"))
        block.vector(mk("dve"))
        block.gpsimd(mk("pool"))
        block.sync(mk("sp"))


class Geo:
    def __init__(self, n_cores=8, nt_kv=4, nt_halo=5, nt_own=16, layers=(0, 1, 2, 3), mixer_only=False):
        self.mixer_only = mixer_only
        self.n_cores = n_cores
        self.nt_kv, self.nt_halo, self.nt_own = nt_kv, nt_halo, nt_own
        self.nt_all = nt_kv + nt_halo + nt_own
        self.own0 = nt_kv + nt_halo
        self.layers = tuple(layers)
        self.T = self.nt_all * 128

    def proc0(self, li):
        last_attn = max([k for k, l in enumerate(self.layers) if l % 3 == 0 and k > 0], default=None)
        if last_attn is not None and li >= last_attn:
            return self.own0
        return self.nt_kv if (last_attn is not None) else self.own0


class DramT:
    def __init__(self, nc, name, shape, dtype, ntiles, kind="Internal"):
        self.h = nc.dram_tensor(name, list(shape), dtype, kind=kind)
        self.b = [Buf("%s_%d" % (name, i), acc=True) for i in range(max(1, ntiles))]


class Prog:
    def __init__(self, geo):
        self.geo = geo
        self.nc = bass.Bass("TRN2", target_bir_lowering=False)
        self.root = ExitStack()
        self.K = Sched(self.nc, self.root)
        self.uid = 0
        self.st = None
        self.stage_bufs = []
        self.ins = {}

    def begin(self):
        self.st = ExitStack()
        self.stage_bufs = []

    def end(self):
        self.K.barrier()
        for b in self.stage_bufs:
            self.K.release_dsem(b)
        self.st.close()
        self.st = None

    def sb(self, name, shape, dtype, nbuf=1):
        self.uid += 1
        t = self.st.enter_context(self.nc.sbuf_tensor("%s_%d" % (name, self.uid), list(shape), dtype))
        bs = [Buf("%s_%d_%d" % (name, self.uid, i)) for i in range(nbuf)]
        self.stage_bufs.extend(bs)
        return (t, bs[0]) if nbuf == 1 else (t, bs)

    def ps(self, name, shape, dtype):
        self.uid += 1
        per_bank = 512 if dtype == F32 else 1024
        ncol = shape[1]
        full = ((ncol + per_bank - 1) // per_bank) * per_bank
        t = self.st.enter_context(self.nc.psum_tensor("%s_%d" % (name, self.uid), [128, full], dtype))
        b = Buf("%s_%d" % (name, self.uid))
        self.stage_bufs.append(b)
        return t[:, 0:ncol], b

    def newbuf(self, name):
        b = Buf(name)
        self.stage_bufs.append(b)
        return b

    def inp(self, name, shape, dtype=F32):
        h = self.nc.dram_tensor(name, list(shape), dtype, kind="ExternalInput")
        self.ins[name] = h
        return h


def _cp(eng_i):
    return "act" if (eng_i % 2) else "dve"


def evac(K, eng, out_ap, in_ap, reads, writes):
    if eng == "act":
        K.op("act", lambda e: e.copy(out=out_ap, in_=in_ap), reads, writes)
    else:
        K.op(eng, lambda e: e.tensor_copy(out=out_ap, in_=in_ap), reads, writes)


def load_ident(P):
    t, b = P.sb("ident", [128, 128], BF16)
    P.K.dma("pool", lambda e: e.dma_start(out=t[:, :], in_=P.ins["cst_ident"][:, :]), b, writes=[b])
    return t, b


class LNRes:
    pass


def ln_setup(P, g_ap, b_ap, nbuf=2):
    R = LNRes()
    R.g, R.gb = P.sb("ln_g", [128, D], F32)
    R.b, R.bb = P.sb("ln_b", [128, D], F32)
    P.K.dma("sp", lambda e: e.dma_start(out=R.g[:, :], in_=g_ap.partition_broadcast(128)), R.gb, writes=[R.gb])
    P.K.dma("sp", lambda e: e.dma_start(out=R.b[:, :], in_=b_ap.partition_broadcast(128)), R.bb, writes=[R.bb])
    R.yb = [P.sb("ln_yb", [128, D], BF16) for _ in range(nbuf)]
    R.xt = [P.sb("ln_xt", [128, KC, 128], BF16) for _ in range(nbuf)]
    R.stat = [P.sb("ln_stat", [128, 64], F32) for _ in range(nbuf)]
    R.nbuf = nbuf
    R.mh, R.mhb = P.sb("ln_mh", [128, 1], F32)
    P.K.op("pool", lambda e: e.memset(R.mh[:, :], -0.5), [], [R.mhb])
    R.pt = [P.ps("ln_pt", [128, 1024], BF16) for _ in range(2)]
    R.ident, R.identb = load_ident(P)
    R.n = 0
    return R


def ln_tile(P, R, s, sbuf, ti, out_x=None, out_xb=None, out_xT=None, act=None, final_out=None):
    K = P.K
    i = R.n % R.nbuf
    R.n += 1
    stat, statb = R.stat[i]
    for c in range(8):
        K.op("dve", lambda e, c=c: e.bn_stats(out=stat[:, c * 6:(c + 1) * 6], in_=s[:, c * 512:(c + 1) * 512]),
             [sbuf], [statb])
    K.op("dve", lambda e: e.bn_aggr(out=stat[:, 48:50], in_=stat[:, 0:48]), [statb], [statb])
    K.op("dve", lambda e: e.tensor_scalar_add(out=stat[:, 50:51], in0=stat[:, 49:50], scalar1=EPS), [statb], [statb])
    K.op("pool", lambda e: e.tensor_tensor(out=stat[:, 51:52], in0=stat[:, 50:51], in1=R.mh[:, :], op=ALU.pow),
         [statb, R.mhb], [statb])
    K.op("dve", lambda e: e.tensor_scalar(out=s[:, :], in0=s[:, :], scalar1=stat[:, 48:49], scalar2=stat[:, 51:52],
                                          op0=ALU.subtract, op1=ALU.mult), [sbuf, statb], [sbuf])
    K.op("dve", lambda e: e.tensor_tensor(out=s[:, :], in0=s[:, :], in1=R.g[:, :], op=ALU.mult), [sbuf, R.gb], [sbuf])
    K.op("pool", lambda e: e.tensor_tensor(out=s[:, :], in0=s[:, :], in1=R.b[:, :], op=ALU.add), [sbuf, R.bb], [sbuf])
    rows = slice(ti * 128, (ti + 1) * 128)
    if final_out is not None:
        h, r0 = final_out
        K.dma("sp", lambda e: e.dma_start(out=h[r0:r0 + 128, :], in_=s[:, :]), sbuf, reads=[sbuf])
    if out_x is not None:
        K.dma("sp", lambda e: e.dma_start(out=out_x.h[rows, :], in_=s[:, :]), sbuf, reads=[sbuf], writes=[out_x.b[ti]])
    if out_xb is None and out_xT is None:
        return
    yb, ybb = R.yb[i]
    if act is None:
        K.op("act", lambda e: e.copy(out=yb[:, :], in_=s[:, :]), [sbuf], [ybb])
    else:
        K.op("act", lambda e: e.activation(out=yb[:, :], in_=s[:, :], func=act), [sbuf], [ybb])
    if out_xb is not None:
        K.dma("sp", lambda e: e.dma_start(out=out_xb.h[rows, :], in_=yb[:, :]), ybb, reads=[ybb], writes=[out_xb.b[ti]])
    if out_xT is not None:
        xt, xtb = R.xt[i]
        for q in range(4):
            pt, ptb = R.pt[q % 2]
            for j in range(8):
                c = q * 8 + j
                K.op("pe", lambda e, c=c, j=j, pt=pt: e.transpose(out=pt[:, j * 128:(j + 1) * 128],
                                                                  in_=yb[:, c * 128:(c + 1) * 128], identity=R.ident[:, :]),
                     [ybb, R.identb], [ptb])
            evac(K, _cp(q), xt[:, q * 8:(q + 1) * 8, :], pt[:, :].rearrange("p (c t) -> p c t", c=8), [ptb], [xtb])
        K.dma("sp", lambda e: e.dma_start(out=out_xT.h[ti, :, :, :], in_=xt[:, :, :]), xtb, reads=[xtb], writes=[out_xT.b[ti]])


def stage_ln(P, tiles, f_src, x_src, g_ap, b_ap, out_x, out_xb, out_xT, bias_ap=None, act=None, alpha=ALPHA,
             final_out=None):
    K = P.K
    P.begin()
    R = ln_setup(P, g_ap, b_ap)
    fs = [P.sb("ln_f", [128, D], F32) for _ in range(2)]
    xs = [P.sb("ln_x", [128, D], F32) for _ in range(2)] if x_src is not None else None
    if bias_ap is not None:
        bt, btb = P.sb("ln_bias", [128, D], F32)
        K.dma("sp", lambda e: e.dma_start(out=bt[:, :], in_=bias_ap.partition_broadcast(128)), btb, writes=[btb])
    for n, ti in enumerate(tiles):
        f, fb = fs[n % 2]
        rows = slice(ti * 128, (ti + 1) * 128)
        K.dma("sp", lambda e, f=f, rows=rows: e.dma_start(out=f[:, :], in_=f_src.h[rows, :]), fb,
              reads=[f_src.b[ti]], writes=[fb])
        if x_src is not None:
            x, xb_ = xs[n % 2]
            K.dma("sp", lambda e, x=x, rows=rows: e.dma_start(out=x[:, :], in_=x_src.h[rows, :]), xb_,
                  reads=[x_src.b[ti]], writes=[xb_])
            K.op("dve", lambda e, f=f, x=x: e.scalar_tensor_tensor(out=f[:, :], in0=x[:, :], scalar=alpha, in1=f[:, :],
                                                                   op0=ALU.mult, op1=ALU.add), [xb_, fb], [fb])
        if bias_ap is not None:
            K.op("pool", lambda e, f=f: e.tensor_tensor(out=f[:, :], in0=f[:, :], in1=bt[:, :], op=ALU.add), [fb, btb], [fb])
        fo = None
        if final_out is not None and ti >= P.geo.own0:
            fo = (final_out, (ti - P.geo.own0) * 128)
        ln_tile(P, R, f, fb, ti, out_x=out_x, out_xb=out_xb, out_xT=out_xT, act=act, final_out=fo)
    P.end()


def stage_prep(P, tiles, x_in, out_xb, out_xT):
    K = P.K
    P.begin()
    ident, identb = load_ident(P)
    xs = [P.sb("pp_x", [128, D], F32) for _ in range(2)]
    ybs = [P.sb("pp_yb", [128, D], BF16) for _ in range(2)]
    xts = [P.sb("pp_xt", [128, KC, 128], BF16) for _ in range(2)]
    pts = [P.ps("pp_pt", [128, 1024], BF16) for _ in range(2)]
    for n, ti in enumerate(tiles):
        x, xb_ = xs[n % 2]
        yb, ybb = ybs[n % 2]
        xt, xtb = xts[n % 2]
        rows = slice(ti * 128, (ti + 1) * 128)
        K.dma("sp", lambda e, x=x, rows=rows: e.dma_start(out=x[:, :], in_=x_in.h[rows, :]), xb_,
              reads=[x_in.b[ti]], writes=[xb_])
        K.op("act", lambda e, x=x, yb=yb: e.copy(out=yb[:, :], in_=x[:, :]), [xb_], [ybb])
        if out_xb is not None:
            K.dma("sp", lambda e, yb=yb, rows=rows: e.dma_start(out=out_xb.h[rows, :], in_=yb[:, :]), ybb,
                  reads=[ybb], writes=[out_xb.b[ti]])
        for q in range(4):
            pt, ptb = pts[q % 2]
            for j in range(8):
                c = q * 8 + j
                K.op("pe", lambda e, c=c, j=j, pt=pt, yb=yb: e.transpose(out=pt[:, j * 128:(j + 1) * 128],
                                                                         in_=yb[:, c * 128:(c + 1) * 128], identity=ident[:, :]),
                     [ybb, identb], [ptb])
            evac(K, _cp(q), xt[:, q * 8:(q + 1) * 8, :], pt[:, :].rearrange("p (c t) -> p c t", c=8), [ptb], [xtb])
        K.dma("sp", lambda e, xt=xt, ti=ti: e.dma_start(out=out_xT.h[ti, :, :, :], in_=xt[:, :, :]), xtb,
              reads=[xtb], writes=[out_xT.b[ti]])
    P.end()


NTS = 9


def stage_gemm(P, tiles, xT_src, w_h, ncols, out_dst, out_dtype, col_off=0):
    K = P.K
    P.begin()
    CB = 512
    ncb = ncols // CB
    XS, xsb = P.sb("g_xs", [128, KC, NTS * 128], BF16, nbuf=NTS)
    wbs = [P.sb("g_wb", [128, KC, CB], BF16) for _ in range(2)]
    oss = [P.sb("g_os", [128, CB], out_dtype) for _ in range(4)]
    pss = [P.ps("g_ps", [128, CB], F32) for _ in range(4)]
    wv = w_h[:, :].rearrange("(k p) n -> p k n", p=128)
    groups = [tiles[i:i + NTS] for i in range(0, len(tiles), NTS)]
    nblk = 0
    nps = 0

    def load_w(cb, slot):
        w, wb_ = wbs[slot]
        for q in range(4):
            K.dma("pool", lambda e, w=w, cb=cb, q=q: e.dma_start(
                out=w[:, q * 8:(q + 1) * 8, :], in_=wv[:, q * 8:(q + 1) * 8, col_off + cb * CB:col_off + (cb + 1) * CB]),
                wb_, writes=[wb_])

    for grp in groups:
        for s, ti in enumerate(grp):
            K.dma("sp", lambda e, s=s, ti=ti: e.dma_start(out=XS[:, :, s * 128:(s + 1) * 128], in_=xT_src.h[ti, :, :, :]),
                  xsb[s], reads=[xT_src.b[ti]], writes=[xsb[s]])
        load_w(0, nblk % 2)
        for cb in range(ncb):
            if cb + 1 < ncb:
                load_w(cb + 1, (nblk + 1) % 2)
            w, wb_ = wbs[nblk % 2]
            for s, ti in enumerate(grp):
                ps, psb = pss[nps % 4]
                os_, osb = oss[nps % 4]
                for k in range(KC):
                    K.op("pe", lambda e, ps=ps, s=s, k=k, w=w: e.matmul(ps[:, :], lhsT=XS[:, k, s * 128:(s + 1) * 128],
                                                                          rhs=w[:, k, :], start=(k == 0), stop=(k == KC - 1)),
                         [xsb[s], wb_], [psb])
                evac(K, _cp(nps), os_[:, :], ps[:, :], [psb], [osb])
                K.dma("sp", lambda e, os_=os_, ti=ti, cb=cb: e.dma_start(
                    out=out_dst.h[ti * 128:(ti + 1) * 128, cb * CB:(cb + 1) * CB], in_=os_[:, :]),
                    osb, reads=[osb], writes=[out_dst.b[ti]])
                nps += 1
            nblk += 1
    P.end()


def stage_attn(P, qtiles, kv0, qkv, bias_h, out_aT):
    K = P.K
    geo = P.geo
    P.begin()
    nt_all = geo.nt_all
    nkv = nt_all - kv0
    nq = len(qtiles)
    q0 = qtiles[0]
    scale = 128.0 ** -0.5
    ident, identb = load_ident(P)
    bandm, bandmb = P.sb("a_bandm", [128, 640], F32)
    K.dma("sp", lambda e: e.dma_start(out=bandm[:, :], in_=P.ins["cst_bandmask"][:, :]), bandmb, writes=[bandmb])
    kmask, kmaskb = P.sb("a_kmask", [128, nt_all * 128], BF16)
    K.op("pool", lambda e: e.memset(kmask[:, :], 0.0), [], [kmaskb])
    K.dma("pool", lambda e: e.dma_start(out=kmask[0:1, :], in_=P.ins["kmask"][:, :]), kmaskb, reads=[kmaskb], writes=[kmaskb])
    ones, onesb = P.sb("a_ones", [128, 128], BF16)
    K.op("pool", lambda e: e.memset(ones[:, :], 0.0), [], [onesb])
    K.op("pool", lambda e: e.memset(ones[0:1, :], 1.0), [onesb], [onesb])
    HG = 2
    grp = [dict(q=P.sb("a_q", [128, nq, HG * 128], BF16), k=P.sb("a_k", [128, nkv, HG * 128], BF16),
                v=P.sb("a_v", [128, nkv, HG * 128], BF16)) for _ in range(2)]
    kts = [P.sb("a_kt", [128, nkv * 128], BF16) for _ in range(2)]
    qts = [P.sb("a_qt", [128, nq * 128], BF16) for _ in range(2)]
    biass = [P.sb("a_bias", [128, 640], F32) for _ in range(2)]
    ots = [P.sb("a_ot", [128, nq, 128], BF16) for _ in range(2)]
    ssb = [P.sb("a_s", [128, 640], F32) for _ in range(2)]
    pbs = [P.sb("a_p", [128, 640], BF16) for _ in range(2)]
    pts = [P.sb("a_pt", [128, 640], BF16) for _ in range(2)]
    osb_ = [P.sb("a_o", [128, 128], BF16) for _ in range(2)]
    sts = [P.sb("a_st", [128, 8], F32) for _ in range(2)]
    ps_s = [P.ps("a_pss", [128, 1024], F32) for _ in range(2)]
    ps_t = [P.ps("a_pst", [128, 1024], BF16) for _ in range(2)]
    pso_t, pso_b = P.ps("a_pso", [128, 512], F32)
    ps_o2 = [(pso_t[:, 0:128], pso_b), (pso_t[:, 128:256], pso_b)]
    ps_m = [P.ps("a_psm", [128, 1024], BF16) for _ in range(1)]
    ps_m2 = [(ps_m[0][0][:, 768:896], ps_m[0][1]), (ps_m[0][0][:, 896:1024], ps_m[0][1])]
    qv = qkv.h[:, :].rearrange("(n p) c -> p n c", p=128)

    def load_group(g, slot):
        G_ = grp[slot]
        c0 = g * HG * 128
        qt_, qb = G_["q"]
        kt_, kb = G_["k"]
        vt_, vb = G_["v"]
        K.dma("sp", lambda e: e.dma_start(out=qt_[:, :, :], in_=qv[:, q0:q0 + nq, c0:c0 + HG * 128]), qb,
              reads=[qkv.b[t] for t in qtiles], writes=[qb])
        K.dma("sp", lambda e: e.dma_start(out=kt_[:, :, :], in_=qv[:, kv0:kv0 + nkv, D + c0:D + c0 + HG * 128]), kb,
              reads=[qkv.b[t] for t in range(kv0, nt_all)], writes=[kb])
        K.dma("sp", lambda e: e.dma_start(out=vt_[:, :, :], in_=qv[:, kv0:kv0 + nkv, 2 * D + c0:2 * D + c0 + HG * 128]), vb,
              reads=[qkv.b[t] for t in range(kv0, nt_all)], writes=[vb])

    ngrp = 32 // HG
    load_group(0, 0)
    cnt = 0
    for g in range(ngrp):
        if g + 1 < ngrp:
            load_group(g + 1, (g + 1) % 2)
        G_ = grp[g % 2]
        qt_, qb = G_["q"]
        kt_, kb = G_["k"]
        vt_, vb = G_["v"]
        for hl in range(HG):
            h = g * HG + hl
            hs = h % 2
            kT, kTb = kts[hs]
            qT, qTb = qts[hs]
            bias, biasb = biass[hs]
            ot, otb = ots[hs]
            K.dma("sp", lambda e, bias=bias, h=h: e.dma_start(out=bias[:, :], in_=bias_h[h, :, :]), biasb, writes=[biasb])
            K.op("pool", lambda e, bias=bias: e.tensor_tensor(out=bias[:, :], in0=bias[:, :], in1=bandm[:, :], op=ALU.add),
                 [biasb, bandmb], [biasb])
            pm, pmb = ps_m[0]
            for src, sbf, n_t, dst, dstb in ((kt_, kb, nkv, kT, kTb), (qt_, qb, nq, qT, qTb)):
                for t0 in range(0, n_t, 6):
                    nn = min(6, n_t - t0)
                    for j in range(nn):
                        K.op("pe", lambda e, src=src, t=t0 + j, j=j, hl=hl: e.transpose(
                            out=pm[:, j * 128:(j + 1) * 128], in_=src[:, t, hl * 128:(hl + 1) * 128], identity=ident[:, :]),
                            [sbf, identb], [pmb])
                    evac(K, _cp(cnt), dst[:, t0 * 128:(t0 + nn) * 128], pm[:, 0:nn * 128], [pmb], [dstb])
                    cnt += 1
            def phaseA(qi, ti, i2, qT=qT, kT=kT, bias=bias, biasb=biasb, qTb=qTb, kTb=kTb):
                kb0 = ti - 4 - kv0
                pS, pSb = ps_s[i2]
                c_lo = kb0 * 128
                need_mask = (ti - 4) < geo.own0
                K.op("pe", lambda e, pS=pS, qi=qi, c_lo=c_lo, nm=need_mask, qT=qT, kT=kT: e.matmul(
                    pS[:, 0:512], lhsT=qT[:, qi * 128:(qi + 1) * 128], rhs=kT[:, c_lo:c_lo + 512], start=True, stop=not nm),
                    [qTb, kTb], [pSb])
                if need_mask:
                    K.op("pe", lambda e, pS=pS, c_lo=c_lo: e.matmul(
                        pS[:, 0:512], lhsT=ones[:, :], rhs=kmask[:, (kv0 * 128 + c_lo):(kv0 * 128 + c_lo + 512)],
                        start=False, stop=True), [onesb, kmaskb], [pSb])
                K.op("pe", lambda e, pS=pS, qi=qi, c_lo=c_lo, nm=need_mask, qT=qT, kT=kT: e.matmul(
                    pS[:, 512:640], lhsT=qT[:, qi * 128:(qi + 1) * 128], rhs=kT[:, c_lo + 512:c_lo + 640], start=True, stop=not nm),
                    [qTb, kTb], [pSb])
                if need_mask:
                    K.op("pe", lambda e, pS=pS, c_lo=c_lo: e.matmul(
                        pS[:, 512:640], lhsT=ones[:, :], rhs=kmask[:, (kv0 * 128 + c_lo + 512):(kv0 * 128 + c_lo + 640)],
                        start=False, stop=True), [onesb, kmaskb], [pSb])
                s_, s_b = ssb[i2]
                st, stb = sts[i2]
                K.op("dve", lambda e, s_=s_, pS=pS, bias=bias: e.scalar_tensor_tensor(
                    out=s_[:, :], in0=pS[:, 0:640], scalar=scale, in1=bias[:, :], op0=ALU.mult, op1=ALU.add),
                    [pSb, biasb], [s_b])
                K.op("dve", lambda e, s_=s_, st=st: e.reduce_max(out=st[:, 1:2], in_=s_[:, :], axis=AX.X, negate=True), [s_b], [stb])
                p_, p_b = pbs[i2]
                K.op("act", lambda e, p_=p_, s_=s_, st=st: e.activation(out=p_[:, :], in_=s_[:, :], func=AF.Exp,
                                                                          bias=st[:, 1:2], scale=1.0, accum_out=st[:, 2:3]),
                     [s_b, stb], [p_b, stb])

            def phaseB(qi, ti, i2, vt_=vt_, vb=vb, hl=hl, ot=ot, otb=otb):
                kb0 = ti - 4 - kv0
                st, stb = sts[i2]
                p_, p_b = pbs[i2]
                pT, pTb = ps_t[i2]
                for j in range(5):
                    K.op("pe", lambda e, pT=pT, p_=p_, j=j: e.transpose(out=pT[:, j * 128:(j + 1) * 128],
                                                                        in_=p_[:, j * 128:(j + 1) * 128], identity=ident[:, :]),
                         [p_b, identb], [pTb])
                pt_, pt_b = pts[i2]
                evac(K, "act", pt_[:, :], pT[:, 0:640], [pTb], [pt_b])
                pO, pOb = ps_o2[i2]
                for j in range(5):
                    K.op("pe", lambda e, pO=pO, pt_=pt_, j=j, kb0=kb0, hl=hl, vt_=vt_: e.matmul(
                        pO, lhsT=pt_[:, j * 128:(j + 1) * 128], rhs=vt_[:, kb0 + j, hl * 128:(hl + 1) * 128],
                        start=(j == 0), stop=(j == 4)), [pt_b, vb], [pOb])
                K.op("dve", lambda e, st=st: e.reciprocal(out=st[:, 3:4], in_=st[:, 2:3]), [stb], [stb])
                o_, o_b = osb_[i2]
                K.op("dve", lambda e, pO=pO, o_=o_, st=st: e.tensor_scalar_mul(out=o_[:, :], in0=pO, scalar1=st[:, 3:4]),
                     [pOb, stb], [o_b])
                pmo, pmob = ps_m2[i2]
                K.op("pe", lambda e, pmo=pmo, o_=o_: e.transpose(out=pmo, in_=o_[:, :], identity=ident[:, :]),
                     [o_b, identb], [pmob])
                evac(K, "dve", ot[:, qi, :], pmo, [pmob], [otb])

            its = [(qi, ti, (cnt + qi) % 2) for qi, ti in enumerate(qtiles)]
            cnt += len(its)
            phaseA(*its[0])
            for n_, it in enumerate(its):
                if n_ + 1 < len(its):
                    phaseA(*its[n_ + 1])
                phaseB(*it)
            K.dma("sp", lambda e, ot=ot, h=h: e.dma_start(
                out=out_aT.h[q0:q0 + nq, :, h, :].rearrange("n p t -> p n t"), in_=ot[:, :, :]), otb,
                reads=[otb], writes=[out_aT.b[t] for t in qtiles])
    P.end()


def stage_memkv(P, wkv_h, mkv, mkt):
    K = P.K
    P.begin()
    ident, identb = load_ident(P)
    memb, membb = P.sb("k_mem", [128, 2, D], BF16)
    K.dma("pool", lambda e: e.dma_start(out=memb[:, 0, :], in_=P.ins["mem"][0:128, :]), membb, writes=[membb])
    K.dma("pool", lambda e: e.dma_start(out=memb[:, 1, :], in_=P.ins["mem"][128:256, :]), membb, writes=[membb])
    memT, memTb = P.sb("k_memT", [128, KC, 256], BF16)
    pm, pmb = P.ps("k_pm", [128, 1024], BF16)
    cnt = 0
    for mt in range(2):
        for q in range(4):
            for j in range(8):
                c = q * 8 + j
                K.op("pe", lambda e, mt=mt, c=c, j=j: e.transpose(out=pm[:, j * 128:(j + 1) * 128],
                                                                    in_=memb[:, mt, c * 128:(c + 1) * 128], identity=ident[:, :]),
                     [membb, identb], [pmb])
            evac(K, _cp(cnt), memT[:, q * 8:(q + 1) * 8, mt * 128:(mt + 1) * 128],
                 pm[:, :].rearrange("p (c t) -> p c t", c=8), [pmb], [memTb])
            cnt += 1
    kvs, kvsb = P.sb("k_kv", [128, 2, 1024], BF16)
    wkvv = wkv_h[:, :].rearrange("(k p) n -> p k n", p=128)
    pkv, pkvb = P.ps("k_pkv", [128, 512], F32)
    wkvs = [P.sb("k_wkv", [128, KC, 512], BF16) for _ in range(2)]
    for half in range(2):
        wk, wkb = wkvs[half]
        for q in range(4):
            K.dma("pool", lambda e, q=q, wk=wk, half=half: e.dma_start(
                out=wk[:, q * 8:(q + 1) * 8, :], in_=wkvv[:, q * 8:(q + 1) * 8, half * 512:(half + 1) * 512]), wkb, writes=[wkb])
        for mt in range(2):
            for k in range(KC):
                K.op("pe", lambda e, k=k, mt=mt, wk=wk: e.matmul(pkv[:, :], lhsT=memT[:, k, mt * 128:(mt + 1) * 128],
                                                                   rhs=wk[:, k, :], start=(k == 0), stop=(k == KC - 1)),
                     [memTb, wkb], [pkvb])
            evac(K, "dve", kvs[:, mt, half * 512:(half + 1) * 512], pkv[:, :], [pkvb], [kvsb])
    kT, kTb = P.sb("k_kT", [128, 4, 256], BF16)
    for h in range(4):
        for mt in range(2):
            K.op("pe", lambda e, h=h, mt=mt: e.transpose(out=pm[:, (h * 2 + mt) * 128:(h * 2 + mt + 1) * 128],
                                                          in_=kvs[:, mt, h * 128:(h + 1) * 128], identity=ident[:, :]),
                 [kvsb, identb], [pmb])
    evac(K, "dve", kT[:, :, :], pm[:, :].rearrange("p (h t) -> p h t", h=4), [pmb], [kTb])
    K.dma("sp", lambda e: e.dma_start(out=mkv.h[:, :], in_=kvs[:, :, :].rearrange("p a b -> p (a b)")), kvsb, reads=[kvsb], writes=[mkv.b[0]])
    K.dma("sp", lambda e: e.dma_start(out=mkt.h[:, :], in_=kT[:, :, :].rearrange("p a b -> p (a b)")), kTb, reads=[kTb], writes=[mkt.b[0]])
    P.end()


def stage_mem(P, tiles, x_src, xT_src, wq_h, wo_h, mkv, mkt, g_ap, b_ap, out_x, out_xb, out_xT, final_out=None):
    K = P.K
    P.begin()
    R = ln_setup(P, g_ap, b_ap, nbuf=2)
    ident, identb = R.ident, R.identb
    scale = 128.0 ** -0.5
    wq, wqb = P.sb("m_wq", [128, KC, 512], BF16)
    wo, wob = P.sb("m_wo", [128, 4, D], BF16)
    wqv = wq_h[:, :].rearrange("(k p) n -> p k n", p=128)
    for q in range(4):
        K.dma("pool", lambda e, q=q: e.dma_start(out=wq[:, q * 8:(q + 1) * 8, :], in_=wqv[:, q * 8:(q + 1) * 8, :]), wqb, writes=[wqb])
    wov = wo_h[:, :].rearrange("(k p) n -> p k n", p=128)
    for q in range(4):
        for hh in range(2):
            K.dma("pool", lambda e, q=q, hh=hh: e.dma_start(out=wo[:, q, hh * 2048:(hh + 1) * 2048],
                                                              in_=wov[:, q, hh * 2048:(hh + 1) * 2048]), wob, writes=[wob])
    kvs, kvsb = P.sb("m_kv", [128, 2, 1024], BF16)
    kT, kTb = P.sb("m_kT", [128, 4, 256], BF16)
    K.dma("sp", lambda e: e.dma_start(out=kvs[:, :, :].rearrange("p a b -> p (a b)"), in_=mkv.h[:, :]), kvsb, reads=[mkv.b[0]], writes=[kvsb])
    K.dma("sp", lambda e: e.dma_start(out=kT[:, :, :].rearrange("p a b -> p (a b)"), in_=mkt.h[:, :]), kTb, reads=[mkt.b[0]], writes=[kTb])
    pm, pmb = P.ps("m_pm", [128, 1024], BF16)
    xts = [P.sb("m_xt", [128, KC, 128], BF16) for _ in range(2)]
    xs = [P.sb("m_x", [128, D], F32) for _ in range(2)]
    qTs = [P.sb("m_qT", [128, 512], BF16) for _ in range(2)]
    pq, pqb = P.ps("m_pq", [128, 512], F32)
    pst, pstb = P.ps("m_ps", [128, 512], F32)
    pss = [(pst[:, 0:256], pstb), (pst[:, 256:512], pstb)]
    pbs = [P.sb("m_p", [128, 256], BF16) for _ in range(2)]
    ptsb = [P.sb("m_pt", [128, 256], BF16) for _ in range(2)]
    sts = [P.sb("m_st", [128, 8], F32) for _ in range(2)]
    po, pob = P.ps("m_po", [128, 128], F32)
    osb, osbb = P.sb("m_o", [128, 512], BF16)
    oT, oTb = P.sb("m_oT", [128, 4, 128], BF16)
    pc = [P.ps("m_pc", [128, 512], F32) for _ in range(2)]
    n2 = 0
    for n, ti in enumerate(tiles):
        xt, xtb = xts[n % 2]
        x, xb_ = xs[n % 2]
        rows = slice(ti * 128, (ti + 1) * 128)
        K.dma("sp", lambda e, xt=xt, ti=ti: e.dma_start(out=xt[:, :, :], in_=xT_src.h[ti, :, :, :]), xtb,
              reads=[xT_src.b[ti]], writes=[xtb])
        K.dma("sp", lambda e, x=x, rows=rows: e.dma_start(out=x[:, :], in_=x_src.h[rows, :]), xb_,
              reads=[x_src.b[ti]], writes=[xb_])
        qT, qTb = qTs[n % 2]
        for h in range(4):
            for k in range(KC):
                K.op("pe", lambda e, h=h, k=k, xt=xt: e.matmul(pq[:, h * 128:(h + 1) * 128], lhsT=wq[:, k, h * 128:(h + 1) * 128],
                                                                 rhs=xt[:, k, :], start=(k == 0), stop=(k == KC - 1)),
                     [wqb, xtb], [pqb])
        K.op("act", lambda e, qT=qT: e.activation(out=qT[:, :], in_=pq[:, :], func=AF.Copy, scale=scale), [pqb], [qTb])
        for h in range(4):
            i2 = n2 % 2
            n2 += 1
            pS, pSb = pss[i2]
            K.op("pe", lambda e, pS=pS, qT=qT, h=h: e.matmul(pS, lhsT=qT[:, h * 128:(h + 1) * 128], rhs=kT[:, h, :],
                                                               start=True, stop=True), [qTb, kTb], [pSb])
            st, stb = sts[i2]
            K.op("dve", lambda e, pS=pS, st=st: e.reduce_max(out=st[:, 1:2], in_=pS, axis=AX.X, negate=True), [pSb], [stb])
            p_, p_b = pbs[i2]
            K.op("act", lambda e, p_=p_, pS=pS, st=st: e.activation(out=p_[:, :], in_=pS, func=AF.Exp, bias=st[:, 1:2],
                                                                      scale=1.0, accum_out=st[:, 2:3]), [pSb, stb], [p_b, stb])
            for j in range(2):
                K.op("pe", lambda e, p_=p_, j=j: e.transpose(out=pm[:, j * 128:(j + 1) * 128], in_=p_[:, j * 128:(j + 1) * 128],
                                                              identity=ident[:, :]), [p_b, identb], [pmb])
            pt_, pt_b = ptsb[i2]
            evac(K, "act", pt_[:, :], pm[:, 0:256], [pmb], [pt_b])
            for j in range(2):
                K.op("pe", lambda e, pt_=pt_, j=j, h=h: e.matmul(po[:, :], lhsT=pt_[:, j * 128:(j + 1) * 128],
                                                                   rhs=kvs[:, j, 512 + h * 128:512 + (h + 1) * 128],
                                                                   start=(j == 0), stop=(j == 1)), [pt_b, kvsb], [pob])
            K.op("dve", lambda e, st=st: e.reciprocal(out=st[:, 3:4], in_=st[:, 2:3]), [stb], [stb])
            K.op("dve", lambda e, st=st, h=h: e.tensor_scalar_mul(out=osb[:, h * 128:(h + 1) * 128], in0=po[:, :], scalar1=st[:, 3:4]),
                 [pob, stb], [osbb])
        for h in range(4):
            K.op("pe", lambda e, h=h: e.transpose(out=pm[:, h * 128:(h + 1) * 128], in_=osb[:, h * 128:(h + 1) * 128],
                                                   identity=ident[:, :]), [osbb, identb], [pmb])
        evac(K, "dve", oT[:, :, :], pm[:, 0:512].rearrange("p (h t) -> p h t", h=4), [pmb], [oTb])
        for cb in range(8):
            pcc, pccb = pc[cb % 2]
            for j in range(4):
                K.op("pe", lambda e, pcc=pcc, j=j, cb=cb: e.matmul(pcc[:, :], lhsT=oT[:, j, :], rhs=wo[:, j, cb * 512:(cb + 1) * 512],
                                                                     start=(j == 0), stop=(j == 3)), [oTb, wob], [pccb])
            K.op("dve", lambda e, pcc=pcc, x=x, cb=cb: e.scalar_tensor_tensor(
                out=x[:, cb * 512:(cb + 1) * 512], in0=x[:, cb * 512:(cb + 1) * 512], scalar=ALPHA, in1=pcc[:, :],
                op0=ALU.mult, op1=ALU.add), [pccb, xb_], [xb_])
        fo = None
        if final_out is not None and ti >= P.geo.own0:
            fo = (final_out, (ti - P.geo.own0) * 128)
        ln_tile(P, R, x, xb_, ti, out_x=out_x, out_xb=out_xb, out_xT=out_xT, final_out=fo)
    P.end()


def stage_router(P, tiles, xT_src, wr_h, br_h, srec, tslot, ztok):
    K = P.K
    P.begin()
    nt = len(tiles)
    wr, wrb = P.sb("r_w", [128, KC, 36], BF16)
    K.dma("pool", lambda e: e.dma_start(out=wr[:, :, :], in_=wr_h[:, :].rearrange("(k p) n -> p k n", p=128)), wrb, writes=[wrb])
    br, brb = P.sb("r_b", [128, 36], F32)
    K.dma("sp", lambda e: e.dma_start(out=br[:, :], in_=br_h[0:1, :].partition_broadcast(128)), brb, writes=[brb])
    tri, trib = P.sb("r_tri", [128, 128], BF16)
    K.dma("pool", lambda e: e.dma_start(out=tri[:, :], in_=P.ins["cst_tri"][:, :]), trib, writes=[trib])
    ones, onesb = P.sb("r_ones", [128, 128], BF16)
    K.op("pool", lambda e: e.memset(ones[:, :], 1.0), [], [onesb])
    iot, iotb = P.sb("r_iota", [128, 32], F32)
    K.dma("sp", lambda e: e.dma_start(out=iot[:, :], in_=P.ins["cst_iota"][:, :]), iotb, writes=[iotb])
    tokid, tokidb = P.sb("r_tokid", [128, P.geo.nt_all], I32)
    K.dma("sp", lambda e: e.dma_start(out=tokid[:, :], in_=P.ins["cst_tokid"][:, :]), tokidb, writes=[tokidb])
    init, initb = P.sb("r_init", [128, NSLOT // 128 + 1, 2], I32)
    K.op("pool", lambda e: e.memset(init[:, :, :], 0), [], [initb])
    K.op("pool", lambda e: e.memset(init[:, :, 0:1], ztok), [initb], [initb])
    initdone = P.newbuf("r_initdone")
    K.dma("sp", lambda e: e.dma_start(out=srec.h[:, :].rearrange("(p n) c -> p n c", p=128), in_=init[:, :, :]), initb,
          reads=[initb], writes=[srec.b[0], initdone])
    A_all, A_allb = P.sb("r_A", [128, nt, 32], BF16, nbuf=nt)
    xts = [P.sb("r_xt", [128, KC, 128], BF16) for _ in range(2)]
    pl, plb = P.ps("r_pl", [128, 36], F32)
    pr, prb = P.ps("r_pr", [128, 32], F32)
    W = [P.sb("r_work", [128, 256], F32) for _ in range(2)]
    recs = [P.sb("r_rec", [128, 2, 2], I32) for _ in range(2)]
    sls = [P.sb("r_sl", [128, 2], I32) for _ in range(2)]
    for n, ti in enumerate(tiles):
        xt, xtb = xts[n % 2]
        K.dma("sp", lambda e, xt=xt, ti=ti: e.dma_start(out=xt[:, :, :], in_=xT_src.h[ti, :, :, :]), xtb,
              reads=[xT_src.b[ti]], writes=[xtb])
        for k in range(KC):
            K.op("pe", lambda e, xt=xt, k=k: e.matmul(pl[:, :], lhsT=xt[:, k, :], rhs=wr[:, k, :], start=(k == 0), stop=(k == KC - 1)),
                 [xtb, wrb], [plb])
        w, wb_ = W[n % 2]
        L = w[:, 0:36]
        dv = lambda fn, rd=(), wr_=None: K.op("dve", fn, [wb_] + list(rd), [wb_] if wr_ is None else wr_)
        K.op("dve", lambda e, w=w: e.tensor_tensor(out=w[:, 0:36], in0=pl[:, :], in1=br[:, :], op=ALU.add), [plb, brb], [wb_])
        dv(lambda e, w=w: e.reduce_max(out=w[:, 44:45], in_=w[:, 0:4], axis=AX.X))
        dv(lambda e, w=w: e.tensor_scalar(out=w[:, 36:40], in0=w[:, 0:4], scalar1=w[:, 44:45], scalar2=None, op0=ALU.is_equal))
        dv(lambda e, w=w: e.tensor_scalar_mul(out=w[:, 45:46], in0=w[:, 44:45], scalar1=-1.0))
        K.op("act", lambda e, w=w: e.activation(out=w[:, 40:44], in_=w[:, 0:4], func=AF.Exp, bias=w[:, 45:46], scale=1.0,
                                                accum_out=w[:, 46:47]), [wb_], [wb_])
        dv(lambda e, w=w: e.reciprocal(out=w[:, 47:48], in_=w[:, 46:47]))
        dv(lambda e, w=w: e.tensor_scalar_mul(out=w[:, 48:56], in0=w[:, 4:12], scalar1=w[:, 36:37]))
        for g in range(1, 4):
            dv(lambda e, w=w, g=g: e.scalar_tensor_tensor(out=w[:, 48:56], in0=w[:, 4 + g * 8:12 + g * 8], scalar=w[:, 36 + g:37 + g],
                                                          in1=w[:, 48:56], op0=ALU.mult, op1=ALU.add))
        dv(lambda e, w=w: e.max(out=w[:, 56:64], in_=w[:, 48:56]))
        dv(lambda e, w=w: e.tensor_scalar(out=w[:, 64:72], in0=w[:, 48:56], scalar1=w[:, 56:57], scalar2=None, op0=ALU.is_equal))
        dv(lambda e, w=w: e.tensor_scalar(out=w[:, 72:80], in0=w[:, 48:56], scalar1=w[:, 57:58], scalar2=None, op0=ALU.is_equal))
        dv(lambda e, w=w: e.tensor_tensor(out=w[:, 208:209], in0=w[:, 57:58], in1=w[:, 56:57], op=ALU.subtract))
        K.op("act", lambda e, w=w: e.activation(out=w[:, 209:210], in_=w[:, 208:209], func=AF.Exp), [wb_], [wb_])
        dv(lambda e, w=w: e.tensor_scalar_add(out=w[:, 210:211], in0=w[:, 209:210], scalar1=1.0))
        dv(lambda e, w=w: e.reciprocal(out=w[:, 211:212], in_=w[:, 210:211]))
        dv(lambda e, w=w: e.tensor_tensor(out=w[:, 212:213], in0=w[:, 211:212], in1=w[:, 47:48], op=ALU.mult))
        dv(lambda e, w=w: e.tensor_tensor(out=w[:, 213:214], in0=w[:, 47:48], in1=w[:, 212:213], op=ALU.subtract))
        for g in range(4):
            dv(lambda e, w=w, g=g: e.tensor_scalar_mul(out=w[:, 80 + g * 8:88 + g * 8], in0=w[:, 64:72], scalar1=w[:, 36 + g:37 + g]))
            dv(lambda e, w=w, g=g: e.tensor_scalar_mul(out=w[:, 112 + g * 8:120 + g * 8], in0=w[:, 72:80], scalar1=w[:, 36 + g:37 + g]))
        K.op("dve", lambda e, w=w, n=n: e.tensor_tensor(out=A_all[:, n, :], in0=w[:, 80:112], in1=w[:, 112:144], op=ALU.add),
             [wb_], [A_allb[n]])
        K.op("pe", lambda e, n=n: e.matmul(pr[:, :], lhsT=tri[:, :], rhs=A_all[:, n, :], start=True, stop=(n == 0)),
             [trib, A_allb[n]], [prb])
        for m in range(n):
            K.op("pe", lambda e, m=m, n=n: e.matmul(pr[:, :], lhsT=ones[:, :], rhs=A_all[:, m, :], start=False, stop=(m == n - 1)),
                 [onesb, A_allb[m]], [prb])
        K.op("dve", lambda e, w=w: e.tensor_copy(out=w[:, 176:208], in_=pr[:, :]), [prb], [wb_])
        K.op("dve", lambda e, w=w: e.tensor_tensor(out=w[:, 144:176], in0=w[:, 176:208], in1=iot[:, :], op=ALU.add), [wb_, iotb], [wb_])
        for kk in range(2):
            a0 = 80 + kk * 32
            dv(lambda e, w=w, a0=a0: e.tensor_tensor(out=w[:, 216:248], in0=w[:, a0:a0 + 32], in1=w[:, 176:208], op=ALU.mult))
            dv(lambda e, w=w, kk=kk: e.reduce_sum(out=w[:, 248 + kk:249 + kk], in_=w[:, 216:248], axis=AX.X))
            dv(lambda e, w=w, a0=a0: e.tensor_tensor(out=w[:, 216:248], in0=w[:, a0:a0 + 32], in1=w[:, 144:176], op=ALU.mult))
            dv(lambda e, w=w, kk=kk: e.reduce_sum(out=w[:, 250 + kk:251 + kk], in_=w[:, 216:248], axis=AX.X))
            dv(lambda e, w=w, kk=kk: e.tensor_scalar(out=w[:, 252 + kk:253 + kk], in0=w[:, 248 + kk:249 + kk], scalar1=float(CAP) - 0.5,
                                                     scalar2=None, op0=ALU.is_ge))
            dv(lambda e, w=w, kk=kk: e.tensor_scalar(out=w[:, 254 + kk:255 + kk], in0=w[:, 250 + kk:251 + kk], scalar1=-1.0,
                                                     scalar2=float(NSLOT), op0=ALU.mult, op1=ALU.add))
            dv(lambda e, w=w, kk=kk: e.tensor_tensor(out=w[:, 254 + kk:255 + kk], in0=w[:, 254 + kk:255 + kk],
                                                     in1=w[:, 252 + kk:253 + kk], op=ALU.mult))
            dv(lambda e, w=w, kk=kk: e.tensor_tensor(out=w[:, 250 + kk:251 + kk], in0=w[:, 250 + kk:251 + kk],
                                                     in1=w[:, 254 + kk:255 + kk], op=ALU.add))
        sl, slb = sls[n % 2]
        K.op("dve", lambda e, w=w, sl=sl: e.tensor_copy(out=sl[:, :], in_=w[:, 250:252]), [wb_], [slb])
        rec, recb = recs[n % 2]
        for kk in range(2):
            K.op("dve", lambda e, rec=rec, kk=kk, ti=ti: e.tensor_copy(out=rec[:, kk, 0:1], in_=tokid[:, ti:ti + 1]), [tokidb], [recb])
            K.op("dve", lambda e, rec=rec, kk=kk, w=w: e.tensor_copy(out=rec[:, kk, 1:2].bitcast(F32), in_=w[:, 212 + kk:213 + kk]),
                 [wb_], [recb])
        K.dma("sp", lambda e, sl=sl, ti=ti: e.dma_start(out=tslot.h[ti * 128:(ti + 1) * 128, :], in_=sl[:, :]), slb,
              reads=[slb], writes=[tslot.b[ti]])
        for kk in range(2):
            K.dma("pool", lambda e, rec=rec, sl=sl, kk=kk: e.indirect_dma_start(
                out=srec.h[:, :], out_offset=bass.IndirectOffsetOnAxis(ap=sl[:, kk:kk + 1], axis=0),
                in_=rec[:, kk, :], in_offset=None), recb, reads=[recb, slb, initdone], writes=[srec.b[0]])
    P.end()


def stage_experts(P, xb_src, srec, wg_h, wu_h, wd_h, ys):
    K = P.K
    P.begin()
    ident, identb = load_ident(P)
    wg_t = [P.sb("e_wg", [128, KC, DEXP], BF16, nbuf=4) for _ in range(2)]
    wu_t = [P.sb("e_wu", [128, KC, DEXP], BF16, nbuf=4) for _ in range(2)]
    wd_t = [P.sb("e_wd", [128, 3, D], BF16, nbuf=6) for _ in range(2)]
    NSTG = 2
    stg = [P.sb("e_stg", [128, 3072], F32) for _ in range(NSTG)]
    xes = [P.sb("e_xe", [128, D], BF16) for _ in range(1)]
    xeTs = [P.sb("e_xeT", [128, KC, 128], BF16) for _ in range(1)]
    recs = [P.sb("e_rec", [128, 2], I32) for _ in range(2)]
    sgs = [P.sb("e_sg", [128, DEXP], F32) for _ in range(2)]
    us = [P.sb("e_u", [128, DEXP], BF16) for _ in range(2)]
    uTs = [P.sb("e_uT", [128, 3, 128], BF16) for _ in range(2)]
    NY = 3
    yss = [P.sb("e_y", [128, 512], F32) for _ in range(NY)]
    pts = [P.ps("e_pt", [128, 1024], BF16) for _ in range(2)]
    ph1 = P.ps("e_ph1", [128, 512], F32)
    ph2 = P.ps("e_ph2", [128, 512], F32)
    pys = [P.ps("e_py", [128, 512], F32) for _ in range(2)]
    z, zb = yss[0]
    K.op("pool", lambda e: e.memset(z[:, :], 0.0), [], [zb])
    for cb in range(8):
        K.dma("pool", lambda e, cb=cb: e.dma_start(out=ys.h[NSLOT:NSLOT + 128, cb * 512:(cb + 1) * 512], in_=z[:, :]), zb,
              reads=[zb], writes=[ys.b[0]])
    for x_, xb__ in xes:
        K.op("pool", lambda e, x_=x_: e.memset(x_[:, :], 0.0), [], [xb__])
    ceng = ["act", "dve"]
    state = dict(ni=0, nc=0)
    pend = []

    def pieces_of(ex):
        out = []
        slot = ex % 2
        for src, (dst, dbufs) in ((wg_h, wg_t[slot]), (wu_h, wu_t[slot])):
            v = src[ex, :, :].rearrange("(k p) n -> p k n", p=128)
            for q in range(4):
                out.append((v[:, q * 8:(q + 1) * 8, :], 8, dst[:, q * 8:(q + 1) * 8, :], dbufs[q], ex - 2))
        v = wd_h[ex, :, :].rearrange("(k p) n -> p k n", p=128)
        wd, wdbs = wd_t[slot]
        for k in range(3):
            for hh in range(2):
                out.append((v[:, k, hh * 2048:(hh + 1) * 2048], None, wd[:, k, hh * 2048:(hh + 1) * 2048], wdbs[k * 2 + hh], ex - 2))
        return out

    allp = []
    for ex in range(NEXP):
        allp.extend(pieces_of(ex))

    def issue():
        while state["ni"] < len(allp) and len(pend) < NSTG:
            src_ap, ksplit, dst_ap, dst_buf, rdy = allp[state["ni"]]
            st, stb = stg[state["ni"] % NSTG]
            sv = st[:, 0:2048] if ksplit is None else st[:, :].rearrange("p (k n) -> p k n", k=ksplit)
            K.dma("sp", lambda e, sv=sv, src_ap=src_ap: e.dma_start(out=sv, in_=src_ap), stb, writes=[stb])
            pend.append((sv, stb, dst_ap, dst_buf, rdy))
            state["ni"] += 1

    def step(done_ex):
        issue()
        if pend and pend[0][4] <= done_ex:
            sv, stb, dst_ap, dst_buf, rdy = pend.pop(0)
            evac(K, ceng[state["nc"] % 2], dst_ap, sv, [stb], [dst_buf])
            state["nc"] += 1
            issue()
            return True
        return False

    for _ in range(14):
        step(-1)
    nb = 0
    ny = 0
    for ex in range(NEXP):
        wg, wgbs = wg_t[ex % 2]
        wu, wubs = wu_t[ex % 2]
        wd, wdbs = wd_t[ex % 2]
        for c in range(CAPB):
            for _ in range(3):
                step(ex - 1)
            s0 = ex * CAP + c * 128
            i2 = nb % 2
            nb += 1
            rec, recb = recs[i2]
            K.dma("pool", lambda e, rec=rec, s0=s0: e.dma_start(out=rec[:, :], in_=srec.h[s0:s0 + 128, :]), recb,
                  reads=[srec.b[0]], writes=[recb])
            xe, xeb = xes[0]
            K.dma("pool", lambda e, xe=xe, rec=rec: e.indirect_dma_start(
                out=xe[:, :], out_offset=None, in_=xb_src.h[:, :],
                in_offset=bass.IndirectOffsetOnAxis(ap=rec[:, 0:1], axis=0)), xeb,
                reads=[recb] + xb_src.b, writes=[xeb])
            xeT, xeTb = xeTs[0]
            for q in range(4):
                pt, ptb = pts[q % 2]
                for j in range(8):
                    cc = q * 8 + j
                    K.op("pe", lambda e, pt=pt, xe=xe, cc=cc, j=j: e.transpose(out=pt[:, j * 128:(j + 1) * 128],
                                                                               in_=xe[:, cc * 128:(cc + 1) * 128], identity=ident[:, :]),
                         [xeb, identb], [ptb])
                evac(K, _cp(q), xeT[:, q * 8:(q + 1) * 8, :], pt[:, :].rearrange("p (c t) -> p c t", c=8), [ptb], [xeTb])
            for (ph, phb), (wt, wtbs) in ((ph1, (wg, wgbs)), (ph2, (wu, wubs))):
                for k in range(KC):
                    K.op("pe", lambda e, ph=ph, xeT=xeT, wt=wt, k=k: e.matmul(ph[:, 0:DEXP], lhsT=xeT[:, k, :], rhs=wt[:, k, :],
                                                                              start=(k == 0), stop=(k == KC - 1)),
                         [xeTb, wtbs[k // 8]], [phb])
            sg, sgb = sgs[i2]
            K.op("act", lambda e, sg=sg: e.activation(out=sg[:, :], in_=ph1[0][:, 0:DEXP], func=AF.Silu), [ph1[1]], [sgb])
            u, ub = us[i2]
            K.op("dve", lambda e, u=u, sg=sg, rec=rec: e.scalar_tensor_tensor(
                out=u[:, :], in0=ph2[0][:, 0:DEXP], scalar=rec[:, 1:2].bitcast(F32), in1=sg[:, :], op0=ALU.mult, op1=ALU.mult),
                [ph2[1], recb, sgb], [ub])
            uT, uTb = uTs[i2]
            pt, ptb = pts[0]
            for j in range(3):
                K.op("pe", lambda e, pt=pt, u=u, j=j: e.transpose(out=pt[:, j * 128:(j + 1) * 128], in_=u[:, j * 128:(j + 1) * 128],
                                                                  identity=ident[:, :]), [ub, identb], [ptb])
            evac(K, "act", uT[:, :, :], pt[:, 0:384].rearrange("p (c t) -> p c t", c=3), [ptb], [uTb])
            for cb in range(8):
                py, pyb = pys[cb % 2]
                for j in range(3):
                    K.op("pe", lambda e, py=py, uT=uT, j=j, cb=cb, wd=wd: e.matmul(py[:, :], lhsT=uT[:, j, :], rhs=wd[:, j, cb * 512:(cb + 1) * 512],
                                                                                   start=(j == 0), stop=(j == 2)), [uTb, wdbs[j * 2 + cb // 4]], [pyb])
                y, yb_ = yss[ny % NY]
                evac(K, _cp(ny), y[:, :], py[:, :], [pyb], [yb_])
                K.dma("pool", lambda e, y=y, s0=s0, cb=cb: e.dma_start(out=ys.h[s0:s0 + 128, cb * 512:(cb + 1) * 512], in_=y[:, :]), yb_,
                      reads=[yb_], writes=[ys.b[0]])
                ny += 1
                if cb % 4 == 3:
                    step(ex - 1)
    P.end()


def stage_combine(P, tiles, x_src, ys, tslot, g_ap, b_ap, out_x, out_xb, out_xT, final_out=None):
    K = P.K
    P.begin()
    R = ln_setup(P, g_ap, b_ap, nbuf=2)
    xs = [P.sb("c_x", [128, D], F32) for _ in range(2)]
    y1s = [P.sb("c_y1", [128, D], F32) for _ in range(2)]
    y2s = [P.sb("c_y2", [128, D], F32) for _ in range(2)]
    sls = [P.sb("c_sl", [128, 2], I32) for _ in range(2)]
    for n, ti in enumerate(tiles):
        x, xb_ = xs[n % 2]
        y1, y1b = y1s[n % 2]
        y2, y2b = y2s[n % 2]
        sl, slb = sls[n % 2]
        rows = slice(ti * 128, (ti + 1) * 128)
        K.dma("sp", lambda e, sl=sl, rows=rows: e.dma_start(out=sl[:, :], in_=tslot.h[rows, :]), slb, reads=[tslot.b[ti]], writes=[slb])
        K.dma("sp", lambda e, x=x, rows=rows: e.dma_start(out=x[:, :], in_=x_src.h[rows, :]), xb_, reads=[x_src.b[ti]], writes=[xb_])
        for yy, yyb, kk in ((y1, y1b, 0), (y2, y2b, 1)):
            K.dma("pool", lambda e, yy=yy, sl=sl, kk=kk: e.indirect_dma_start(
                out=yy[:, :], out_offset=None, in_=ys.h[:, :], in_offset=bass.IndirectOffsetOnAxis(ap=sl[:, kk:kk + 1], axis=0)),
                yyb, reads=[slb, ys.b[0]], writes=[yyb])
        K.op("dve", lambda e, x=x, y1=y1: e.scalar_tensor_tensor(out=x[:, :], in0=x[:, :], scalar=ALPHA, in1=y1[:, :],
                                                                 op0=ALU.mult, op1=ALU.add), [xb_, y1b], [xb_])
        K.op("pool", lambda e, x=x, y2=y2: e.tensor_tensor(out=x[:, :], in0=x[:, :], in1=y2[:, :], op=ALU.add), [xb_, y2b], [xb_])
        fo = None
        if final_out is not None and ti >= P.geo.own0:
            fo = (final_out, (ti - P.geo.own0) * 128)
        ln_tile(P, R, x, xb_, ti, out_x=out_x, out_xb=out_xb, out_xT=out_xT, final_out=fo)
    P.end()


PADC = 32


def stage_glu(P, tiles, xT_src, win_h, bin_h, ut):
    K = P.K
    geo = P.geo
    P.begin()
    NTG = 8
    CBW = 256
    XS, xsb = P.sb("u_xs", [128, KC, NTG * 128], BF16, nbuf=NTG)
    was = [P.sb("u_wa", [128, KC, CBW], BF16) for _ in range(2)]
    wgs = [P.sb("u_wg", [128, KC, CBW], BF16) for _ in range(2)]
    bin_, binb = P.sb("u_bin", [128, 64], F32)
    K.dma("sp", lambda e: e.dma_start(out=bin_[:, :], in_=bin_h[:, :]), binb, writes=[binb])
    tv, tvb = P.sb("u_tv", [128, geo.nt_all * 128], F32)
    K.dma("sp", lambda e: e.dma_start(out=tv[:, :], in_=P.ins["tokvalid"][0:1, :].partition_broadcast(128)), tvb, writes=[tvb])
    z, zb = P.sb("u_z", [128, 32, PADC], BF16)
    K.op("pool", lambda e: e.memset(z[:, :, :], 0.0), [], [zb])
    K.dma("sp", lambda e: e.dma_start(out=ut.h[:, :, 0:PADC].rearrange("c p t -> p c t"), in_=z[:, :, :]), zb, reads=[zb], writes=ut.b)
    sgs = [P.sb("u_sg", [128, 512], F32) for _ in range(2)]
    uss = [P.sb("u_u", [128, 512], BF16) for _ in range(3)]
    pas = [P.ps("u_pa", [128, 512], F32) for _ in range(2)]
    pgs = [P.ps("u_pg", [128, 512], F32) for _ in range(2)]
    wv = win_h[:, :].rearrange("(k p) n -> p k n", p=128)
    t0 = tiles[0]
    groups = [tiles[i:i + NTG] for i in range(0, len(tiles), NTG)]
    nblk = 0
    nn = 0

    def load_w(cb, slot):
        for (w, wb_), off in ((was[slot], 0), (wgs[slot], D)):
            for q in range(4):
                K.dma("pool", lambda e, w=w, q=q, off=off, cb=cb: e.dma_start(
                    out=w[:, q * 8:(q + 1) * 8, :], in_=wv[:, q * 8:(q + 1) * 8, off + cb * CBW:off + (cb + 1) * CBW]), wb_, writes=[wb_])

    ncb = D // CBW
    for grp in groups:
        for s, ti in enumerate(grp):
            K.dma("sp", lambda e, s=s, ti=ti: e.dma_start(out=XS[:, :, s * 128:(s + 1) * 128], in_=xT_src.h[ti, :, :, :]),
                  xsb[s], reads=[xT_src.b[ti]], writes=[xsb[s]])
        load_w(0, nblk % 2)
        for cb in range(ncb):
            if cb + 1 < ncb:
                load_w(cb + 1, (nblk + 1) % 2)
            wa, wab = was[nblk % 2]
            wg, wgb = wgs[nblk % 2]
            for m in range(CBW // 128):
                ch = cb * (CBW // 128) + m
                for tg in range(0, len(grp), 4):
                    ntk = min(4, len(grp) - tg)
                    ncol = ntk * 128
                    pa, pab = pas[nn % 2]
                    pg, pgb = pgs[nn % 2]
                    rd = [xsb[tg + j] for j in range(ntk)]
                    for (pp, ppb), (w, wb_) in (((pa, pab), (wa, wab)), ((pg, pgb), (wg, wgb))):
                        for k in range(KC):
                            K.op("pe", lambda e, pp=pp, w=w, k=k, m=m, tg=tg, ncol=ncol: e.matmul(
                                pp[:, 0:ncol], lhsT=w[:, k, m * 128:(m + 1) * 128], rhs=XS[:, k, tg * 128:tg * 128 + ncol],
                                start=(k == 0), stop=(k == KC - 1)), rd + [wb_], [ppb])
                    sg, sgb = sgs[nn % 2]
                    u, ub = uss[nn % 3]
                    K.op("act", lambda e, sg=sg, pg=pg, ch=ch, ncol=ncol: e.activation(
                        out=sg[:, 0:ncol], in_=pg[:, 0:ncol], func=AF.Sigmoid, bias=bin_[:, 32 + ch:33 + ch], scale=1.0), [pgb, binb], [sgb])
                    K.op("dve", lambda e, u=u, pa=pa, sg=sg, ch=ch, ncol=ncol: e.scalar_tensor_tensor(
                        out=u[:, 0:ncol], in0=pa[:, 0:ncol], scalar=bin_[:, ch:ch + 1], in1=sg[:, 0:ncol], op0=ALU.add, op1=ALU.mult),
                        [pab, binb, sgb], [ub])
                    tfirst = grp[tg]
                    if tfirst < geo.own0:
                        K.op("pool", lambda e, u=u, tfirst=tfirst, ncol=ncol: e.tensor_tensor(
                            out=u[:, 0:ncol], in0=u[:, 0:ncol], in1=tv[:, tfirst * 128:tfirst * 128 + ncol], op=ALU.mult), [ub, tvb], [ub])
                    c0 = PADC + (tfirst - t0) * 128
                    K.dma("sp", lambda e, u=u, ch=ch, c0=c0, ncol=ncol: e.dma_start(out=ut.h[ch, :, c0:c0 + ncol], in_=u[:, 0:ncol]),
                          ub, reads=[ub], writes=[ut.b[ch]])
                    nn += 1
            nblk += 1
    P.end()


def stage_dwconv(P, tiles, ut, wdw_h, bdw_h, f_dst):
    K = P.K
    P.begin()
    nt = len(tiles)
    Tp = nt * 128
    identb16, identb16b = load_ident(P)
    identf, identfb = P.sb("d_identf", [128, 128], F32)
    K.dma("sp", lambda e: e.dma_start(out=identf[:, :], in_=P.ins["cst_ident"][:, :]), identfb, writes=[identfb])
    wdw, wdwb = P.sb("d_wdw", [128, 32, 31], F32)
    K.dma("sp", lambda e: e.dma_start(out=wdw[:, :, :], in_=wdw_h[:, :, :]), wdwb, writes=[wdwb])
    bdw, bdwb = P.sb("d_bdw", [128, 32], F32)
    K.dma("sp", lambda e: e.dma_start(out=bdw[:, :], in_=bdw_h[:, :]), bdwb, writes=[bdwb])
    uts = [P.sb("d_ut", [128, PADC + Tp], BF16) for _ in range(2)]
    dgs = [P.sb("d_dg", [128, 31, 128], BF16) for _ in range(2)]
    cvs = [P.sb("d_cv", [128, 512], F32) for _ in range(2)]
    ots = [P.sb("d_ot", [128, 4, 128], F32) for _ in range(2)]
    pcs = [P.ps("d_pc", [128, 512], F32) for _ in range(2)]
    pts = [P.ps("d_pt", [128, 512], F32) for _ in range(2)]
    nn = 0

    def load_ut(ch, slot):
        u, ub = uts[slot]
        K.dma("sp", lambda e, u=u, ch=ch: e.dma_start(out=u[:, :], in_=ut.h[ch, :, 0:PADC + Tp]), ub, reads=[ut.b[ch]], writes=[ub])

    load_ut(0, 0)
    for ch in range(32):
        if ch + 1 < 32:
            load_ut(ch + 1, (ch + 1) % 2)
        u, ub = uts[ch % 2]
        dg, dgb = dgs[ch % 2]
        for j in range(31):
            K.op("dve" if j % 2 == 0 else "pool", lambda e, dg=dg, j=j, ch=ch: e.tensor_scalar_mul(
                out=dg[:, j, :], in0=identb16[:, :], scalar1=wdw[:, ch, j:j + 1]), [identb16b, wdwb], [dgb])
        for tg in range(0, nt, 4):
            ntk = min(4, nt - tg)
            ncol = ntk * 128
            pc, pcb = pcs[nn % 2]
            for j in range(31):
                c0 = PADC + tg * 128 - 30 + j
                K.op("pe", lambda e, pc=pc, dg=dg, u=u, j=j, c0=c0, ncol=ncol: e.matmul(
                    pc[:, 0:ncol], lhsT=dg[:, j, :], rhs=u[:, c0:c0 + ncol], start=(j == 0), stop=(j == 30)), [dgb, ub], [pcb])
            cv, cvb = cvs[nn % 2]
            K.op("act", lambda e, cv=cv, pc=pc, ch=ch, ncol=ncol: e.activation(out=cv[:, 0:ncol], in_=pc[:, 0:ncol], func=AF.Identity,
                                                                               bias=bdw[:, ch:ch + 1], scale=1.0), [pcb, bdwb], [cvb])
            pt, ptb = pts[nn % 2]
            for j in range(ntk):
                K.op("pe", lambda e, pt=pt, cv=cv, j=j: e.transpose(out=pt[:, j * 128:(j + 1) * 128], in_=cv[:, j * 128:(j + 1) * 128],
                                                                    identity=identf[:, :]), [cvb, identfb], [ptb])
            ot, otb = ots[nn % 2]
            evac(K, "dve", ot[:, 0:ntk, :], pt[:, 0:ncol].rearrange("p (n c) -> p n c", n=ntk), [ptb], [otb])
            r0 = tiles[tg] * 128
            K.dma("sp", lambda e, ot=ot, r0=r0, ntk=ntk, ch=ch: e.dma_start(
                out=f_dst.h[r0:r0 + ntk * 128, ch * 128:(ch + 1) * 128].rearrange("(n p) c -> p n c", p=128), in_=ot[:, 0:ntk, :]),
                otb, reads=[otb], writes=[f_dst.b[tiles[tg + j]] for j in range(ntk)])
            nn += 1
    P.end()


def stage_pool(P, tiles, xb_src, wp_h, scale_ap, f_dst):
    K = P.K
    geo = P.geo
    P.begin()
    wp, wpb = P.sb("p_wp", [128, 4, 8, 1024], BF16)
    for g in range(4):
        v = wp_h[g, :, :].rearrange("(k p) n -> p k n", p=128)
        for q in range(2):
            K.dma("pool", lambda e, g=g, q=q, v=v: e.dma_start(out=wp[:, g, q * 4:(q + 1) * 4, :], in_=v[:, q * 4:(q + 1) * 4, :]), wpb, writes=[wpb])
    sc, scb = P.sb("p_sc", [128, D], F32)
    K.dma("sp", lambda e: e.dma_start(out=sc[:, :], in_=scale_ap.partition_broadcast(128)), scb, writes=[scb])
    Bg, Bgb = P.sb("p_B", [128, 8, 128], BF16)
    B0, B0b = P.sb("p_B0", [128, 8, 128], BF16)
    K.dma("pool", lambda e: e.dma_start(out=Bg[:, :, :], in_=P.ins["cst_poolB"][:, :, :].rearrange("n p t -> p n t")), Bgb, writes=[Bgb])
    K.dma("pool", lambda e: e.dma_start(out=B0[:, :, :], in_=P.ins["poolB0"][:, :, :].rearrange("n p t -> p n t")), B0b, writes=[B0b])
    xbs = [P.sb("p_xb", [128, D], BF16) for _ in range(3)]
    pTs = [P.sb("p_pT", [128, KC, 128], BF16) for _ in range(2)]
    fss = [P.sb("p_f", [128, 512], F32) for _ in range(4)]
    pps = [P.ps("p_pp", [128, 512], F32) for _ in range(2)]
    pys = [P.ps("p_py", [128, 512], F32) for _ in range(2)]
    nn = 0
    nf = 0
    prev = None
    for n, ti in enumerate(tiles):
        xb, xbb = xbs[n % 3]
        rows = slice(ti * 128, (ti + 1) * 128)
        K.dma("sp", lambda e, xb=xb, rows=rows: e.dma_start(out=xb[:, :], in_=xb_src.h[rows, :]), xbb, reads=[xb_src.b[ti]], writes=[xbb])
        Bm, Bmb = (B0, B0b) if ti == geo.own0 else (Bg, Bgb)
        pT, pTb = pTs[n % 2]
        for q in range(8):
            pp, ppb = pps[nn % 2]
            nn += 1
            for j in range(4):
                c = q * 4 + j
                g = c // 8
                K.op("pe", lambda e, pp=pp, xb=xb, c=c, j=j, g=g, Bm=Bm: e.matmul(
                    pp[:, j * 128:(j + 1) * 128], lhsT=xb[:, c * 128:(c + 1) * 128], rhs=Bm[:, g * 2, :], start=True, stop=(prev is None)),
                    [xbb, Bmb], [ppb])
                if prev is not None:
                    pxb, pxbb = prev
                    K.op("pe", lambda e, pp=pp, pxb=pxb, c=c, j=j, g=g, Bm=Bm: e.matmul(
                        pp[:, j * 128:(j + 1) * 128], lhsT=pxb[:, c * 128:(c + 1) * 128], rhs=Bm[:, g * 2 + 1, :], start=False, stop=True),
                        [pxbb, Bmb], [ppb])
            evac(K, _cp(q), pT[:, q * 4:(q + 1) * 4, :], pp[:, :].rearrange("p (c t) -> p c t", c=4), [ppb], [pTb])
        for g in range(4):
            for cb in range(2):
                py, pyb = pys[nf % 2]
                for k in range(8):
                    K.op("pe", lambda e, py=py, pT=pT, g=g, k=k, cb=cb: e.matmul(
                        py[:, :], lhsT=pT[:, g * 8 + k, :], rhs=wp[:, g, k, cb * 512:(cb + 1) * 512], start=(k == 0), stop=(k == 7)),
                        [pTb, wpb], [pyb])
                f, fb = fss[nf % 4]
                col = g * 1024 + cb * 512
                K.op("dve", lambda e, f=f, py=py, col=col: e.tensor_tensor(out=f[:, :], in0=py[:, :], in1=sc[:, col:col + 512], op=ALU.mult),
                     [pyb, scb], [fb])
                K.dma("sp", lambda e, f=f, rows=rows, col=col: e.dma_start(out=f_dst.h[rows, col:col + 512], in_=f[:, :]), fb,
                      reads=[fb], writes=[f_dst.b[ti]])
                nf += 1
        prev = (xb, xbb)
    P.end()


def build(geo):
    P = Prog(geo)
    nc = P.nc
    K = P.K
    NT = geo.nt_all
    T = NT * 128
    layers = geo.layers
    x_in = DramT(nc, "x_ext", [T, D], F32, NT, kind="ExternalInput")
    P.ins["x_ext"] = x_in.h
    P.inp("mem", [256, D])
    P.inp("kmask", [1, T])
    P.inp("tokvalid", [1, T])
    P.inp("poolB0", [8, 128, 128])
    P.inp("cst_ident", [128, 128])
    P.inp("cst_tri", [128, 128])
    P.inp("cst_iota", [128, 32])
    P.inp("cst_tokid", [128, NT], I32)
    P.inp("cst_bandmask", [128, 640])
    P.inp("cst_poolB", [8, 128, 128])
    W = {}
    for l in layers:
        kind = l % 3
        if kind == 0:
            W["wqkv", l] = P.inp("wqkv%d" % l, [D, 3 * D])
            W["wo", l] = P.inp("wo%d" % l, [D, D])
            W["abias", l] = P.inp("abias%d" % l, [32, 128, 640])
        elif kind == 1:
            W["win", l] = P.inp("win%d" % l, [D, 2 * D])
            W["bin", l] = P.inp("bin%d" % l, [128, 64])
            W["wdw", l] = P.inp("wdw%d" % l, [128, 32, 31])
            W["bdw", l] = P.inp("bdw%d" % l, [128, 32])
            W["cg", l] = P.inp("cg%d" % l, [1, D])
            W["cb", l] = P.inp("cb%d" % l, [1, D])
            W["wout", l] = P.inp("wout%d" % l, [D, D])
            W["bout", l] = P.inp("bout%d" % l, [1, D])
        else:
            W["wp", l] = P.inp("wp%d" % l, [4, 1024, 1024])
            W["psc", l] = P.inp("psc%d" % l, [1, D])
        W["wq", l] = P.inp("wq%d" % l, [D, 512])
        W["wkv", l] = P.inp("wkv%d" % l, [D, 1024])
        W["wmo", l] = P.inp("wmo%d" % l, [512, D])
        W["wr", l] = P.inp("wr%d" % l, [D, 36])
        W["br", l] = P.inp("br%d" % l, [1, 36])
        W["wg", l] = P.inp("wg%d" % l, [NEXP, D, DEXP])
        W["wu", l] = P.inp("wu%d" % l, [NEXP, D, DEXP])
        W["wd", l] = P.inp("wd%d" % l, [NEXP, DEXP, D])
        W["lng", l] = P.inp("lng%d" % l, [3, D])
        W["lnb", l] = P.inp("lnb%d" % l, [3, D])
    out_h = nc.dram_tensor("out", [geo.nt_own * 128, D], F32, kind="ExternalOutput")
    X = [DramT(nc, "X%d" % i, [T, D], F32, NT) for i in range(2)]
    XB = [DramT(nc, "XB%d" % i, [T + 128, D], BF16, NT) for i in range(2)]
    XT = [DramT(nc, "XT%d" % i, [NT, 128, KC, 128], BF16, NT) for i in range(2)]
    Fd = DramT(nc, "F", [T, D], F32, NT)
    QKV = DramT(nc, "QKV", [T, 3 * D], BF16, NT)
    AT = DramT(nc, "AT", [NT, 128, KC, 128], BF16, NT)
    UT = DramT(nc, "UT", [32, 128, PADC + T], BF16, 32)
    YS = DramT(nc, "YS", [NSLOT + 128, D], F32, 1)
    SREC = DramT(nc, "SREC", [NSLOT + 128, 2], I32, 1)
    TSLOT = DramT(nc, "TSLOT", [T, 2], I32, NT)
    MKV = DramT(nc, "MKV", [128, 2048], BF16, 1)
    MKT = DramT(nc, "MKT", [128, 1024], BF16, 1)

    attn_pos = [k for k, l in enumerate(layers) if l % 3 == 0]
    last_attn = attn_pos[-1] if (attn_pos and attn_pos[-1] > 0) else None
    def proc0(k):
        if geo.nt_halo == 0:
            return geo.own0
        if last_attn is not None and k >= last_attn:
            return geo.own0
        return geo.nt_kv if last_attn is not None else geo.own0

    P.begin()
    z, zb = P.sb("z0", [128, D], BF16)
    K.op("pool", lambda e: e.memset(z[:, :], 0.0), [], [zb])
    for i in range(2):
        K.dma("sp", lambda e, i=i: e.dma_start(out=XB[i].h[T:T + 128, :], in_=z[:, :]), zb, reads=[zb], writes=XB[i].b)
        for ti in range(geo.nt_kv):
            K.dma("sp", lambda e, i=i, ti=ti: e.dma_start(out=XT[i].h[ti, :, :, :], in_=z[:, :].rearrange("p (c t) -> p c t", c=KC)),
                  zb, reads=[zb], writes=[XT[i].b[ti]])
    P.end()

    first_kv0 = proc0(0) - 4 if layers[0] % 3 == 0 else proc0(0)
    stage_prep(P, list(range(first_kv0, NT)), x_in, XB[0], XT[0])
    cur = 0
    xres = x_in
    nl = len(layers)
    for k, l in enumerate(layers):
        kind = l % 3
        tiles = list(range(proc0(k), NT))
        lng = W["lng", l]
        lnb = W["lnb", l]
        nxt = 1 - cur
        if kind == 0:
            kv0 = tiles[0] - 4
            stage_gemm(P, list(range(kv0, NT)), XT[cur], W["wqkv", l], 3 * D, QKV, BF16)
            stage_attn(P, tiles, kv0, QKV, W["abias", l], AT)
            stage_gemm(P, tiles, AT, W["wo", l], D, Fd, F32)
            stage_ln(P, tiles, Fd, xres, lng[0:1, :], lnb[0:1, :], X[nxt], XB[nxt], XT[nxt],
                     final_out=out_h if geo.mixer_only else None)
        elif kind == 1:
            stage_glu(P, tiles, XT[cur], W["win", l], W["bin", l], UT)
            stage_dwconv(P, tiles, UT, W["wdw", l], W["bdw", l], Fd)
            stage_ln(P, tiles, Fd, None, W["cg", l][0:1, :], W["cb", l][0:1, :], None, None, AT, act=AF.Silu)
            stage_gemm(P, tiles, AT, W["wout", l], D, Fd, F32)
            stage_ln(P, tiles, Fd, xres, lng[0:1, :], lnb[0:1, :], X[nxt], XB[nxt], XT[nxt], bias_ap=W["bout", l][0:1, :],
                     final_out=out_h if geo.mixer_only else None)
        else:
            stage_pool(P, tiles, XB[cur], W["wp", l], W["psc", l][0:1, :], Fd)
            stage_ln(P, tiles, Fd, xres, lng[0:1, :], lnb[0:1, :], X[nxt], XB[nxt], XT[nxt],
                     final_out=out_h if geo.mixer_only else None)
        cur = nxt
        xres = X[cur]
        nxt = 1 - cur
        if geo.mixer_only:
            continue
        stage_memkv(P, W["wkv", l], MKV, MKT)
        stage_mem(P, tiles, xres, XT[cur], W["wq", l], W["wmo", l], MKV, MKT, lng[1:2, :], lnb[1:2, :], X[nxt], XB[nxt], XT[nxt])
        cur = nxt
        xres = X[cur]
        nxt = 1 - cur
        stage_router(P, tiles, XT[cur], W["wr", l], W["br", l], SREC, TSLOT, T)
        stage_experts(P, XB[cur], SREC, W["wg", l], W["wu", l], W["wd", l], YS)
        last = (k == nl - 1)
        stage_combine(P, tiles, xres, YS, TSLOT, lng[2:3, :], lnb[2:3, :], X[nxt], XB[nxt], XT[nxt],
                      final_out=out_h if last else None)
        cur = nxt
        xres = X[cur]
    with nc.Block() as block:
        K.emit(block)
    P.root.close()
    return P


def host_consts(geo):
    NT = geo.nt_all
    c = {}
    c["cst_ident"] = np.eye(128, dtype=np.float32)
    tp = np.arange(128)
    c["cst_tri"] = (tp[:, None] < tp[None, :]).astype(np.float32)
    c["cst_iota"] = np.tile((np.arange(32, dtype=np.float32) * CAP)[None, :], (128, 1))
    c["cst_tokid"] = (np.arange(NT, dtype=np.int32)[None, :] * 128 + np.arange(128, dtype=np.int32)[:, None]).astype(np.int32)
    q = np.arange(128)[:, None]
    j = np.arange(640)[None, :]
    bm = np.zeros((128, 640), np.float32)
    bm[(q < 64) & (j >= 576)] = NEG
    bm[(q >= 64) & (j < 64)] = NEG
    c["cst_bandmask"] = bm
    wins = (2, 4, 8, 16)
    B = np.zeros((8, 128, 128), np.float32)
    t1 = np.arange(128)[:, None]
    t = np.arange(128)[None, :]
    for g, w in enumerate(wins):
        B[g * 2] = ((t1 <= t) & (t1 > t - w)).astype(np.float32) / w - (t1 == t).astype(np.float32)
        B[g * 2 + 1] = ((t1 - 128) > (t - w)).astype(np.float32) / w
    c["cst_poolB"] = B
    return c


def host_poolB0(core, consts):
    if core != 0:
        return consts["cst_poolB"]
    wins = (2, 4, 8, 16)
    B = np.zeros((8, 128, 128), np.float32)
    t1 = np.arange(128)[:, None]
    t = np.arange(128)[None, :]
    for g, w in enumerate(wins):
        cnt = np.minimum(t + 1, w).astype(np.float32)
        B[g * 2] = ((t1 <= t) & (t1 > t - w)).astype(np.float32) / cnt - (t1 == t).astype(np.float32)
    return B


def host_layer_inputs(l, prm):
    kind, j = l % 3, l // 3
    d = {}
    if kind == 0:
        d["wqkv%d" % l] = prm["attn_w_qkv"][j]
        d["wo%d" % l] = prm["attn_w_o"][j]
        q = np.arange(128)[:, None]
        jj = np.arange(640)[None, :]
        idx = np.clip(512 + q - jj, -128, 128) + 128
        d["abias%d" % l] = np.ascontiguousarray(prm["attn_rel_bias"][j][:, idx])
    elif kind == 1:
        d["win%d" % l] = prm["conv_w_in"][j]
        d["bin%d" % l] = np.ascontiguousarray(prm["conv_b_in"][j].reshape(64, 128).T)
        d["wdw%d" % l] = np.ascontiguousarray(prm["conv_w_dw"][j].reshape(31, 32, 128).transpose(2, 1, 0))
        d["bdw%d" % l] = np.ascontiguousarray(prm["conv_b_dw"][j].reshape(32, 128).T)
        d["cg%d" % l] = prm["conv_ln_g"][j].reshape(1, D)
        d["cb%d" % l] = prm["conv_ln_b"][j].reshape(1, D)
        d["wout%d" % l] = prm["conv_w_out"][j]
        d["bout%d" % l] = prm["conv_b_out"][j].reshape(1, D)
    else:
        d["wp%d" % l] = prm["pool_w"][j]
        d["psc%d" % l] = prm["pool_scale"][j].reshape(1, D)
    d["wq%d" % l] = prm["mem_w_q"][l]
    d["wkv%d" % l] = prm["mem_w_kv"][l]
    d["wmo%d" % l] = prm["mem_w_o"][l]
    d["wr%d" % l] = np.ascontiguousarray(np.concatenate(
        [prm["moe_w_group"][l], prm["moe_w_router"][l].transpose(1, 0, 2).reshape(D, 32)], axis=1))
    d["br%d" % l] = np.concatenate([prm["moe_b_group"][l], prm["moe_b_router"][l].reshape(32)]).reshape(1, 36).astype(np.float32)
    d["wg%d" % l] = prm["moe_w_gate"][l]
    d["wu%d" % l] = prm["moe_w_up"][l]
    d["wd%d" % l] = prm["moe_w_down"][l]
    d["lng%d" % l] = prm["ln_g"][l]
    d["lnb%d" % l] = prm["ln_b"][l]
    return d


def host_core_inputs(geo, core, x2d, shared):
    NT = geo.nt_all
    T = NT * 128
    own = geo.nt_own * 128
    start = core * own - geo.own0 * 128
    pos = start + np.arange(T)
    valid = pos >= 0
    xe = np.zeros((T, D), np.float32)
    xe[valid] = x2d[pos[valid]]
    d = dict(shared)
    d["x_ext"] = xe
    d["kmask"] = np.where(valid, 0.0, NEG).astype(np.float32).reshape(1, T)
    d["tokvalid"] = valid.astype(np.float32).reshape(1, T)
    d["poolB0"] = host_poolB0(core, shared)
    return d


_CACHE = {}


def run_geo(geo, x2d, mem2d, prm):
    key = (geo.n_cores, geo.nt_kv, geo.nt_halo, geo.nt_own, geo.layers, geo.mixer_only)
    if key not in _CACHE:
        _CACHE[key] = build(geo)
    P = _CACHE[key]
    shared = host_consts(geo)
    shared["mem"] = np.ascontiguousarray(mem2d, dtype=np.float32)
    for l in geo.layers:
        shared.update(host_layer_inputs(l, prm))
    in_maps = [host_core_inputs(geo, c, x2d, shared) for c in range(geo.n_cores)]
    res = run_bass_kernel_spmd(P.nc, in_maps, core_ids=list(range(geo.n_cores)))
    return np.concatenate([r["out"] for r in res.results], axis=0)


def kernel(**inputs):
    prm = {k: np.asarray(v) for k, v in inputs.items()}
    x = prm.pop("x")
    mem = prm.pop("mem")
    geo = Geo(n_cores=8, nt_kv=4, nt_halo=5, nt_own=16, layers=(0, 1, 2, 3))
    out = run_geo(geo, x[0], mem[0], prm)
    return out.reshape(1, out.shape[0], D).astype(np.float32)
```

```python
import numpy as np
from contextlib import ExitStack
import concourse.bass as bass
import concourse.mybir as mybir
from concourse.bass_utils import run_bass_kernel_spmd

F32 = mybir.dt.float32
BF16 = mybir.dt.bfloat16
I32 = mybir.dt.int32
AF = mybir.ActivationFunctionType
ALU = mybir.AluOpType
AX = mybir.AxisListType

ENGS = ["pe", "act", "dve", "pool", "sp"]
NDMA = 90

D = 4096
KC = 32
ALPHA = 8.0 ** 0.25
EPS = 1e-5
NEG = -30000.0
DEXP = 384
NEXP = 32
CAPB = 3
CAP = CAPB * 128
NSLOT = NEXP * CAP


class Buf:
    __slots__ = ("name", "w", "r", "acc", "dsem")

    def __init__(self, name, acc=False):
        self.name = name
        self.w = {}
        self.r = {}
        self.acc = acc
        self.dsem = None


class Sched:
    def __init__(self, nc, stack):
        self.nc = nc
        self.sems = []
        self.eng = {}
        for name in ENGS:
            h = stack.enter_context(nc.semaphore("s_" + name))
            self.sems.append(h)
            self.eng[name] = dict(sem=len(self.sems) - 1, cnt=0, seen={}, prog=[], pending={})
        self.dma_sems = []
        for i in range(NDMA):
            h = stack.enter_context(nc.semaphore("d%d" % i))
            self.sems.append(h)
            self.dma_sems.append(len(self.sems) - 1)
        self.dma_val = {s: 0 for s in self.dma_sems}
        self.dma_free = list(self.dma_sems)
        self.n_ops = 0

    def dsem_of(self, buf):
        if buf.dsem is None:
            buf.dsem = self.dma_free.pop(0)
        return buf.dsem

    def release_dsem(self, buf):
        if buf.dsem is not None:
            self.dma_free.append(buf.dsem)
            buf.dsem = None

    def barrier(self):
        snap = {}
        for name in ENGS:
            E = self.eng[name]
            if E["cnt"] > 0:
                snap[E["sem"]] = E["cnt"]
        for s, v in self.dma_val.items():
            if v > 0:
                snap[s] = v
        for name in ENGS:
            E = self.eng[name]
            for s, v in snap.items():
                if E["pending"].get(s, 0) < v:
                    E["pending"][s] = v

    def _waits(self, E, own, reads, writes, skip_own):
        waits = dict(E["pending"])
        E["pending"] = {}
        for b in reads:
            for s, v in b.w.items():
                if v > waits.get(s, 0):
                    waits[s] = v
        for b in writes:
            if not b.acc:
                for s, v in b.w.items():
                    if v > waits.get(s, 0):
                        waits[s] = v
            for s, v in b.r.items():
                if v > waits.get(s, 0):
                    waits[s] = v
        need = []
        seen = E["seen"]
        for s, v in waits.items():
            if s == own and skip_own:
                continue
            if seen.get(s, 0) < v:
                seen[s] = v
                need.append((s, v))
        return need

    def op(self, eng, fn, reads=(), writes=()):
        E = self.eng[eng]
        own = E["sem"]
        need = self._waits(E, own, reads, writes, skip_own=(eng == "pe"))
        E["cnt"] += 1
        v = E["cnt"]
        E["prog"].append((need, fn, own, 1))
        for b in reads:
            if b.r.get(own, 0) < v:
                b.r[own] = v
        for b in writes:
            if b.acc:
                b.w[own] = v
            else:
                b.w = {own: v}
                b.r = {}
        self.n_ops += 1

    def dma(self, q, fn, sbuf, reads=(), writes=()):
        E = self.eng[q]
        d = self.dsem_of(sbuf)
        wr = []
        join = set()
        for b in writes:
            if (not b.acc) and len(b.r) == 0 and len(b.w) > 0 and set(b.w.keys()) <= {d}:
                join.add(id(b))
                continue
            wr.append(b)
        need = self._waits(E, None, reads, wr, skip_own=False)
        self.dma_val[d] += 16
        v = self.dma_val[d]
        E["prog"].append((need, fn, d, 16))
        for b in reads:
            if b.r.get(d, 0) < v:
                b.r[d] = v
        for b in writes:
            if b.acc or id(b) in join:
                b.w[d] = v
            else:
                b.w = {d: v}
                b.r = {}
        self.n_ops += 1

    def emit(self, block):
        sems = self.sems
        final = []
        for name in ENGS:
            E = self.eng[name]
            if name != "sp" and E["cnt"] > 0:
                final.append((E["sem"], E["cnt"]))
        for s, v in self.dma_val.items():
            if v > 0:
                final.append((s, v))

        def mk(name):
            E = self.eng[name]

            def body(eng):
                for need, fn, s, inc in E["prog"]:
                    for ws, wv in need:
                        eng.wait_ge(sems[ws], wv)
                    fn(eng).then_inc(sems[s], inc)
                if name == "sp":
                    for ws, wv in final:
                        eng.wait_ge(sems[ws], wv)
            return body

        block.tensor(mk("pe"))
        block.scalar(mk("act"))
        block.vector(mk("dve"))
        block.gpsimd(mk("pool"))
        block.sync(mk("sp"))


class Geo:
    def __init__(self, n_cores=8, nt_kv=4, nt_halo=5, nt_own=16, layers=(0, 1, 2, 3), mixer_only=False):
        self.mixer_only = mixer_only
        self.n_cores = n_cores
        self.nt_kv, self.nt_halo, self.nt_own = nt_kv, nt_halo, nt_own
        self.nt_all = nt_kv + nt_halo + nt_own
        self.own0 = nt_kv + nt_halo
        self.layers = tuple(layers)
        self.T = self.nt_all * 128

    def proc0(self, li):
        last_attn = max([k for k, l in enumerate(self.layers) if l % 3 == 0 and k > 0], default=None)
        if last_attn is not None and li >= last_attn:
            return self.own0
        return self.nt_kv if (last_attn is not None) else self.own0


class DramT:
    def __init__(self, nc, name, shape, dtype, ntiles, kind="Internal"):
        self.h = nc.dram_tensor(name, list(shape), dtype, kind=kind)
        self.b = [Buf("%s_%d" % (name, i), acc=True) for i in range(max(1, ntiles))]


class Prog:
    def __init__(self, geo):
        self.geo = geo
        self.nc = bass.Bass("TRN2", target_bir_lowering=False)
        self.root = ExitStack()
        self.K = Sched(self.nc, self.root)
        self.uid = 0
        self.st = None
        self.stage_bufs = []
        self.ins = {}

    def begin(self):
        self.st = ExitStack()
        self.stage_bufs = []

    def end(self):
        self.K.barrier()
        for b in self.stage_bufs:
            self.K.release_dsem(b)
        self.st.close()
        self.st = None

    def sb(self, name, shape, dtype, nbuf=1):
        self.uid += 1
        t = self.st.enter_context(self.nc.sbuf_tensor("%s_%d" % (name, self.uid), list(shape), dtype))
        bs = [Buf("%s_%d_%d" % (name, self.uid, i)) for i in range(nbuf)]
        self.stage_bufs.extend(bs)
        return (t, bs[0]) if nbuf == 1 else (t, bs)

    def ps(self, name, shape, dtype):
        self.uid += 1
        per_bank = 512 if dtype == F32 else 1024
        ncol = shape[1]
        full = ((ncol + per_bank - 1) // per_bank) * per_bank
        t = self.st.enter_context(self.nc.psum_tensor("%s_%d" % (name, self.uid), [128, full], dtype))
        b = Buf("%s_%d" % (name, self.uid))
        self.stage_bufs.append(b)
        return t[:, 0:ncol], b

    def newbuf(self, name):
        b = Buf(name)
        self.stage_bufs.append(b)
        return b

    def inp(self, name, shape, dtype=F32):
        h = self.nc.dram_tensor(name, list(shape), dtype, kind="ExternalInput")
        self.ins[name] = h
        return h


def _cp(eng_i):
    return "act" if (eng_i % 2) else "dve"


def evac(K, eng, out_ap, in_ap, reads, writes):
    if eng == "act":
        K.op("act", lambda e: e.copy(out=out_ap, in_=in_ap), reads, writes)
    else:
        K.op(eng, lambda e: e.tensor_copy(out=out_ap, in_=in_ap), reads, writes)


def load_ident(P):
    t, b = P.sb("ident", [128, 128], BF16)
    P.K.dma("pool", lambda e: e.dma_start(out=t[:, :], in_=P.ins["cst_ident"][:, :]), b, writes=[b])
    return t, b


class LNRes:
    pass


def ln_setup(P, g_ap, b_ap, nbuf=2):
    R = LNRes()
    R.g, R.gb = P.sb("ln_g", [128, D], F32)
    R.b, R.bb = P.sb("ln_b", [128, D], F32)
    P.K.dma("sp", lambda e: e.dma_start(out=R.g[:, :], in_=g_ap.partition_broadcast(128)), R.gb, writes=[R.gb])
    P.K.dma("sp", lambda e: e.dma_start(out=R.b[:, :], in_=b_ap.partition_broadcast(128)), R.bb, writes=[R.bb])
    R.yb = [P.sb("ln_yb", [128, D], BF16) for _ in range(nbuf)]
    R.xt = [P.sb("ln_xt", [128, KC, 128], BF16) for _ in range(nbuf)]
    R.stat = [P.sb("ln_stat", [128, 64], F32) for _ in range(nbuf)]
    R.nbuf = nbuf
    R.mh, R.mhb = P.sb("ln_mh", [128, 1], F32)
    P.K.op("pool", lambda e: e.memset(R.mh[:, :], -0.5), [], [R.mhb])
    R.pt = [P.ps("ln_pt", [128, 1024], BF16) for _ in range(2)]
    R.ident, R.identb = load_ident(P)
    R.n = 0
    return R


def ln_tile(P, R, s, sbuf, ti, out_x=None, out_xb=None, out_xT=None, act=None, final_out=None):
    K = P.K
    i = R.n % R.nbuf
    R.n += 1
    stat, statb = R.stat[i]
    for c in range(8):
        K.op("dve", lambda e, c=c: e.bn_stats(out=stat[:, c * 6:(c + 1) * 6], in_=s[:, c * 512:(c + 1) * 512]),
             [sbuf], [statb])
    K.op("dve", lambda e: e.bn_aggr(out=stat[:, 48:50], in_=stat[:, 0:48]), [statb], [statb])
    K.op("dve", lambda e: e.tensor_scalar_add(out=stat[:, 50:51], in0=stat[:, 49:50], scalar1=EPS), [statb], [statb])
    K.op("pool", lambda e: e.tensor_tensor(out=stat[:, 51:52], in0=stat[:, 50:51], in1=R.mh[:, :], op=ALU.pow),
         [statb, R.mhb], [statb])
    K.op("dve", lambda e: e.tensor_scalar(out=s[:, :], in0=s[:, :], scalar1=stat[:, 48:49], scalar2=stat[:, 51:52],
                                          op0=ALU.subtract, op1=ALU.mult), [sbuf, statb], [sbuf])
    K.op("dve", lambda e: e.tensor_tensor(out=s[:, :], in0=s[:, :], in1=R.g[:, :], op=ALU.mult), [sbuf, R.gb], [sbuf])
    K.op("pool", lambda e: e.tensor_tensor(out=s[:, :], in0=s[:, :], in1=R.b[:, :], op=ALU.add), [sbuf, R.bb], [sbuf])
    rows = slice(ti * 128, (ti + 1) * 128)
    if final_out is not None:
        h, r0 = final_out
        K.dma("sp", lambda e: e.dma_start(out=h[r0:r0 + 128, :], in_=s[:, :]), sbuf, reads=[sbuf])
    if out_x is not None:
        K.dma("sp", lambda e: e.dma_start(out=out_x.h[rows, :], in_=s[:, :]), sbuf, reads=[sbuf], writes=[out_x.b[ti]])
    if out_xb is None and out_xT is None:
        return
    yb, ybb = R.yb[i]
    if act is None:
        K.op("act", lambda e: e.copy(out=yb[:, :], in_=s[:, :]), [sbuf], [ybb])
    else:
        K.op("act", lambda e: e.activation(out=yb[:, :], in_=s[:, :], func=act), [sbuf], [ybb])
    if out_xb is not None:
        K.dma("sp", lambda e: e.dma_start(out=out_xb.h[rows, :], in_=yb[:, :]), ybb, reads=[ybb], writes=[out_xb.b[ti]])
    if out_xT is not None:
        xt, xtb = R.xt[i]
        for q in range(4):
            pt, ptb = R.pt[q % 2]
            for j in range(8):
                c = q * 8 + j
                K.op("pe", lambda e, c=c, j=j, pt=pt: e.transpose(out=pt[:, j * 128:(j + 1) * 128],
                                                                  in_=yb[:, c * 128:(c + 1) * 128], identity=R.ident[:, :]),
                     [ybb, R.identb], [ptb])
            evac(K, _cp(q), xt[:, q * 8:(q + 1) * 8, :], pt[:, :].rearrange("p (c t) -> p c t", c=8), [ptb], [xtb])
        K.dma("sp", lambda e: e.dma_start(out=out_xT.h[ti, :, :, :], in_=xt[:, :, :]), xtb, reads=[xtb], writes=[out_xT.b[ti]])


def stage_ln(P, tiles, f_src, x_src, g_ap, b_ap, out_x, out_xb, out_xT, bias_ap=None, act=None, alpha=ALPHA,
             final_out=None):
    K = P.K
    P.begin()
    R = ln_setup(P, g_ap, b_ap)
    fs = [P.sb("ln_f", [128, D], F32) for _ in range(2)]
    xs = [P.sb("ln_x", [128, D], F32) for _ in range(2)] if x_src is not None else None
    if bias_ap is not None:
        bt, btb = P.sb("ln_bias", [128, D], F32)
        K.dma("sp", lambda e: e.dma_start(out=bt[:, :], in_=bias_ap.partition_broadcast(128)), btb, writes=[btb])
    for n, ti in enumerate(tiles):
        f, fb = fs[n % 2]
        rows = slice(ti * 128, (ti + 1) * 128)
        K.dma("sp", lambda e, f=f, rows=rows: e.dma_start(out=f[:, :], in_=f_src.h[rows, :]), fb,
              reads=[f_src.b[ti]], writes=[fb])
        if x_src is not None:
            x, xb_ = xs[n % 2]
            K.dma("sp", lambda e, x=x, rows=rows: e.dma_start(out=x[:, :], in_=x_src.h[rows, :]), xb_,
                  reads=[x_src.b[ti]], writes=[xb_])
            K.op("dve", lambda e, f=f, x=x: e.scalar_tensor_tensor(out=f[:, :], in0=x[:, :], scalar=alpha, in1=f[:, :],
                                                                   op0=ALU.mult, op1=ALU.add), [xb_, fb], [fb])
        if bias_ap is not None:
            K.op("pool", lambda e, f=f: e.tensor_tensor(out=f[:, :], in0=f[:, :], in1=bt[:, :], op=ALU.add), [fb, btb], [fb])
        fo = None
        if final_out is not None and ti >= P.geo.own0:
            fo = (final_out, (ti - P.geo.own0) * 128)
        ln_tile(P, R, f, fb, ti, out_x=out_x, out_xb=out_xb, out_xT=out_xT, act=act, final_out=fo)
    P.end()


def stage_prep(P, tiles, x_in, out_xb, out_xT):
    K = P.K
    P.begin()
    ident, identb = load_ident(P)
    xs = [P.sb("pp_x", [128, D], F32) for _ in range(2)]
    ybs = [P.sb("pp_yb", [128, D], BF16) for _ in range(2)]
    xts = [P.sb("pp_xt", [128, KC, 128], BF16) for _ in range(2)]
    pts = [P.ps("pp_pt", [128, 1024], BF16) for _ in range(2)]
    for n, ti in enumerate(tiles):
        x, xb_ = xs[n % 2]
        yb, ybb = ybs[n % 2]
        xt, xtb = xts[n % 2]
        rows = slice(ti * 128, (ti + 1) * 128)
        K.dma("sp", lambda e, x=x, rows=rows: e.dma_start(out=x[:, :], in_=x_in.h[rows, :]), xb_,
              reads=[x_in.b[ti]], writes=[xb_])
        K.op("act", lambda e, x=x, yb=yb: e.copy(out=yb[:, :], in_=x[:, :]), [xb_], [ybb])
        if out_xb is not None:
            K.dma("sp", lambda e, yb=yb, rows=rows: e.dma_start(out=out_xb.h[rows, :], in_=yb[:, :]), ybb,
                  reads=[ybb], writes=[out_xb.b[ti]])
        for q in range(4):
            pt, ptb = pts[q % 2]
            for j in range(8):
                c = q * 8 + j
                K.op("pe", lambda e, c=c, j=j, pt=pt, yb=yb: e.transpose(out=pt[:, j * 128:(j + 1) * 128],
                                                                         in_=yb[:, c * 128:(c + 1) * 128], identity=ident[:, :]),
                     [ybb, identb], [ptb])
            evac(K, _cp(q), xt[:, q * 8:(q + 1) * 8, :], pt[:, :].rearrange("p (c t) -> p c t", c=8), [ptb], [xtb])
        K.dma("sp", lambda e, xt=xt, ti=ti: e.dma_start(out=out_xT.h[ti, :, :, :], in_=xt[:, :, :]), xtb,
              reads=[xtb], writes=[out_xT.b[ti]])
    P.end()


NTS = 9


def stage_gemm(P, tiles, xT_src, w_h, ncols, out_dst, out_dtype, col_off=0):
    K = P.K
    P.begin()
    CB = 512
    ncb = ncols // CB
    XS, xsb = P.sb("g_xs", [128, KC, NTS * 128], BF16, nbuf=NTS)
    wbs = [P.sb("g_wb", [128, KC, CB], BF16) for _ in range(2)]
    oss = [P.sb("g_os", [128, CB], out_dtype) for _ in range(4)]
    pss = [P.ps("g_ps", [128, CB], F32) for _ in range(4)]
    wv = w_h[:, :].rearrange("(k p) n -> p k n", p=128)
    groups = [tiles[i:i + NTS] for i in range(0, len(tiles), NTS)]
    nblk = 0
    nps = 0

    def load_w(cb, slot):
        w, wb_ = wbs[slot]
        for q in range(4):
            K.dma("pool", lambda e, w=w, cb=cb, q=q: e.dma_start(
                out=w[:, q * 8:(q + 1) * 8, :], in_=wv[:, q * 8:(q + 1) * 8, col_off + cb * CB:col_off + (cb + 1) * CB]),
                wb_, writes=[wb_])

    for grp in groups:
        for s, ti in enumerate(grp):
            K.dma("sp", lambda e, s=s, ti=ti: e.dma_start(out=XS[:, :, s * 128:(s + 1) * 128], in_=xT_src.h[ti, :, :, :]),
                  xsb[s], reads=[xT_src.b[ti]], writes=[xsb[s]])
        load_w(0, nblk % 2)
        for cb in range(ncb):
            if cb + 1 < ncb:
                load_w(cb + 1, (nblk + 1) % 2)
            w, wb_ = wbs[nblk % 2]
            for s, ti in enumerate(grp):
                ps, psb = pss[nps % 4]
                os_, osb = oss[nps % 4]
                for k in range(KC):
                    K.op("pe", lambda e, ps=ps, s=s, k=k, w=w: e.matmul(ps[:, :], lhsT=XS[:, k, s * 128:(s + 1) * 128],
                                                                          rhs=w[:, k, :], start=(k == 0), stop=(k == KC - 1)),
                         [xsb[s], wb_], [psb])
                evac(K, _cp(nps), os_[:, :], ps[:, :], [psb], [osb])
                K.dma("sp", lambda e, os_=os_, ti=ti, cb=cb: e.dma_start(
                    out=out_dst.h[ti * 128:(ti + 1) * 128, cb * CB:(cb + 1) * CB], in_=os_[:, :]),
                    osb, reads=[osb], writes=[out_dst.b[ti]])
                nps += 1
            nblk += 1
    P.end()


def stage_attn(P, qtiles, kv0, qkv, bias_h, out_aT):
    K = P.K
    geo = P.geo
    P.begin()
    nt_all = geo.nt_all
    nkv = nt_all - kv0
    nq = len(qtiles)
    q0 = qtiles[0]
    scale = 128.0 ** -0.5
    ident, identb = load_ident(P)
    bandm, bandmb = P.sb("a_bandm", [128, 640], F32)
    K.dma("sp", lambda e: e.dma_start(out=bandm[:, :], in_=P.ins["cst_bandmask"][:, :]), bandmb, writes=[bandmb])
    kmask, kmaskb = P.sb("a_kmask", [128, nt_all * 128], BF16)
    K.op("pool", lambda e: e.memset(kmask[:, :], 0.0), [], [kmaskb])
    K.dma("pool", lambda e: e.dma_start(out=kmask[0:1, :], in_=P.ins["kmask"][:, :]), kmaskb, reads=[kmaskb], writes=[kmaskb])
    ones, onesb = P.sb("a_ones", [128, 128], BF16)
    K.op("pool", lambda e: e.memset(ones[:, :], 0.0), [], [onesb])
    K.op("pool", lambda e: e.memset(ones[0:1, :], 1.0), [onesb], [onesb])
    HG = 2
    grp = [dict(q=P.sb("a_q", [128, nq, HG * 128], BF16), k=P.sb("a_k", [128, nkv, HG * 128], BF16),
                v=P.sb("a_v", [128, nkv, HG * 128], BF16)) for _ in range(2)]
    kts = [P.sb("a_kt", [128, nkv * 128], BF16) for _ in range(2)]
    qts = [P.sb("a_qt", [128, nq * 128], BF16) for _ in range(2)]
    biass = [P.sb("a_bias", [128, 640], F32) for _ in range(2)]
    ots = [P.sb("a_ot", [128, nq, 128], BF16) for _ in range(2)]
    ssb = [P.sb("a_s", [128, 640], F32) for _ in range(2)]
    pbs = [P.sb("a_p", [128, 640], BF16) for _ in range(2)]
    pts = [P.sb("a_pt", [128, 640], BF16) for _ in range(2)]
    osb_ = [P.sb("a_o", [128, 128], BF16) for _ in range(2)]
    sts = [P.sb("a_st", [128, 8], F32) for _ in range(2)]
    ps_s = [P.ps("a_pss", [128, 1024], F32) for _ in range(2)]
    ps_t = [P.ps("a_pst", [128, 1024], BF16) for _ in range(2)]
    pso_t, pso_b = P.ps("a_pso", [128, 512], F32)
    ps_o2 = [(pso_t[:, 0:128], pso_b), (pso_t[:, 128:256], pso_b)]
    ps_m = [P.ps("a_psm", [128, 1024], BF16) for _ in range(1)]
    ps_m2 = [(ps_m[0][0][:, 768:896], ps_m[0][1]), (ps_m[0][0][:, 896:1024], ps_m[0][1])]
    qv = qkv.h[:, :].rearrange("(n p) c -> p n c", p=128)

    def load_group(g, slot):
        G_ = grp[slot]
        c0 = g * HG * 128
        qt_, qb = G_["q"]
        kt_, kb = G_["k"]
        vt_, vb = G_["v"]
        K.dma("sp", lambda e: e.dma_start(out=qt_[:, :, :], in_=qv[:, q0:q0 + nq, c0:c0 + HG * 128]), qb,
              reads=[qkv.b[t] for t in qtiles], writes=[qb])
        K.dma("sp", lambda e: e.dma_start(out=kt_[:, :, :], in_=qv[:, kv0:kv0 + nkv, D + c0:D + c0 + HG * 128]), kb,
              reads=[qkv.b[t] for t in range(kv0, nt_all)], writes=[kb])
        K.dma("sp", lambda e: e.dma_start(out=vt_[:, :, :], in_=qv[:, kv0:kv0 + nkv, 2 * D + c0:2 * D + c0 + HG * 128]), vb,
              reads=[qkv.b[t] for t in range(kv0, nt_all)], writes=[vb])

    ngrp = 32 // HG
    load_group(0, 0)
    cnt = 0
    for g in range(ngrp):
        if g + 1 < ngrp:
            load_group(g + 1, (g + 1) % 2)
        G_ = grp[g % 2]
        qt_, qb = G_["q"]
        kt_, kb = G_["k"]
        vt_, vb = G_["v"]
        for hl in range(HG):
            h = g * HG + hl
            hs = h % 2
            kT, kTb = kts[hs]
            qT, qTb = qts[hs]
            bias, biasb = biass[hs]
            ot, otb = ots[hs]
            K.dma("sp", lambda e, bias=bias, h=h: e.dma_start(out=bias[:, :], in_=bias_h[h, :, :]), biasb, writes=[biasb])
            K.op("pool", lambda e, bias=bias: e.tensor_tensor(out=bias[:, :], in0=bias[:, :], in1=bandm[:, :], op=ALU.add),
                 [biasb, bandmb], [biasb])
            pm, pmb = ps_m[0]
            for src, sbf, n_t, dst, dstb in ((kt_, kb, nkv, kT, kTb), (qt_, qb, nq, qT, qTb)):
                for t0 in range(0, n_t, 6):
                    nn = min(6, n_t - t0)
                    for j in range(nn):
                        K.op("pe", lambda e, src=src, t=t0 + j, j=j, hl=hl: e.transpose(
                            out=pm[:, j * 128:(j + 1) * 128], in_=src[:, t, hl * 128:(hl + 1) * 128], identity=ident[:, :]),
                            [sbf, identb], [pmb])
                    evac(K, _cp(cnt), dst[:, t0 * 128:(t0 + nn) * 128], pm[:, 0:nn * 128], [pmb], [dstb])
                    cnt += 1
            def phaseA(qi, ti, i2, qT=qT, kT=kT, bias=bias, biasb=biasb, qTb=qTb, kTb=kTb):
                kb0 = ti - 4 - kv0
                pS, pSb = ps_s[i2]
                c_lo = kb0 * 128
                need_mask = (ti - 4) < geo.own0
                K.op("pe", lambda e, pS=pS, qi=qi, c_lo=c_lo, nm=need_mask, qT=qT, kT=kT: e.matmul(
                    pS[:, 0:512], lhsT=qT[:, qi * 128:(qi + 1) * 128], rhs=kT[:, c_lo:c_lo + 512], start=True, stop=not nm),
                    [qTb, kTb], [pSb])
                if need_mask:
                    K.op("pe", lambda e, pS=pS, c_lo=c_lo: e.matmul(
                        pS[:, 0:512], lhsT=ones[:, :], rhs=kmask[:, (kv0 * 128 + c_lo):(kv0 * 128 + c_lo + 512)],
                        start=False, stop=True), [onesb, kmaskb], [pSb])
                K.op("pe", lambda e, pS=pS, qi=qi, c_lo=c_lo, nm=need_mask, qT=qT, kT=kT: e.matmul(
                    pS[:, 512:640], lhsT=qT[:, qi * 128:(qi + 1) * 128], rhs=kT[:, c_lo + 512:c_lo + 640], start=True, stop=not nm),
                    [qTb, kTb], [pSb])
                if need_mask:
                    K.op("pe", lambda e, pS=pS, c_lo=c_lo: e.matmul(
                        pS[:, 512:640], lhsT=ones[:, :], rhs=kmask[:, (kv0 * 128 + c_lo + 512):(kv0 * 128 + c_lo + 640)],
                        start=False, stop=True), [onesb, kmaskb], [pSb])
                s_, s_b = ssb[i2]
                st, stb = sts[i2]
                K.op("dve", lambda e, s_=s_, pS=pS, bias=bias: e.scalar_tensor_tensor(
                    out=s_[:, :], in0=pS[:, 0:640], scalar=scale, in1=bias[:, :], op0=ALU.mult, op1=ALU.add),
                    [pSb, biasb], [s_b])
                K.op("dve", lambda e, s_=s_, st=st: e.reduce_max(out=st[:, 1:2], in_=s_[:, :], axis=AX.X, negate=True), [s_b], [stb])
                p_, p_b = pbs[i2]
                K.op("act", lambda e, p_=p_, s_=s_, st=st: e.activation(out=p_[:, :], in_=s_[:, :], func=AF.Exp,
                                                                          bias=st[:, 1:2], scale=1.0, accum_out=st[:, 2:3]),
                     [s_b, stb], [p_b, stb])

            def phaseB(qi, ti, i2, vt_=vt_, vb=vb, hl=hl, ot=ot, otb=otb):
                kb0 = ti - 4 - kv0
                st, stb = sts[i2]
                p_, p_b = pbs[i2]
                pT, pTb = ps_t[i2]
                for j in range(5):
                    K.op("pe", lambda e, pT=pT, p_=p_, j=j: e.transpose(out=pT[:, j * 128:(j + 1) * 128],
                                                                        in_=p_[:, j * 128:(j + 1) * 128], identity=ident[:, :]),
                         [p_b, identb], [pTb])
                pt_, pt_b = pts[i2]
                evac(K, "act", pt_[:, :], pT[:, 0:640], [pTb], [pt_b])
                pO, pOb = ps_o2[i2]
                for j in range(5):
                    K.op("pe", lambda e, pO=pO, pt_=pt_, j=j, kb0=kb0, hl=hl, vt_=vt_: e.matmul(
                        pO, lhsT=pt_[:, j * 128:(j + 1) * 128], rhs=vt_[:, kb0 + j, hl * 128:(hl + 1) * 128],
                        start=(j == 0), stop=(j == 4)), [pt_b, vb], [pOb])
                K.op("dve", lambda e, st=st: e.reciprocal(out=st[:, 3:4], in_=st[:, 2:3]), [stb], [stb])
                o_, o_b = osb_[i2]
                K.op("dve", lambda e, pO=pO, o_=o_, st=st: e.tensor_scalar_mul(out=o_[:, :], in0=pO, scalar1=st[:, 3:4]),
                     [pOb, stb], [o_b])
                pmo, pmob = ps_m2[i2]
                K.op("pe", lambda e, pmo=pmo, o_=o_: e.transpose(out=pmo, in_=o_[:, :], identity=ident[:, :]),
                     [o_b, identb], [pmob])
                evac(K, "dve", ot[:, qi, :], pmo, [pmob], [otb])

            its = [(qi, ti, (cnt + qi) % 2) for qi, ti in enumerate(qtiles)]
            cnt += len(its)
            phaseA(*its[0])
            for n_, it in enumerate(its):
                if n_ + 1 < len(its):
                    phaseA(*its[n_ + 1])
                phaseB(*it)
            K.dma("sp", lambda e, ot=ot, h=h: e.dma_start(
                out=out_aT.h[q0:q0 + nq, :, h, :].rearrange("n p t -> p n t"), in_=ot[:, :, :]), otb,
                reads=[otb], writes=[out_aT.b[t] for t in qtiles])
    P.end()


def stage_memkv(P, wkv_h, mkv, mkt):
    K = P.K
    P.begin()
    ident, identb = load_ident(P)
    memb, membb = P.sb("k_mem", [128, 2, D], BF16)
    K.dma("pool", lambda e: e.dma_start(out=memb[:, 0, :], in_=P.ins["mem"][0:128, :]), membb, writes=[membb])
    K.dma("pool", lambda e: e.dma_start(out=memb[:, 1, :], in_=P.ins["mem"][128:256, :]), membb, writes=[membb])
    memT, memTb = P.sb("k_memT", [128, KC, 256], BF16)
    pm, pmb = P.ps("k_pm", [128, 1024], BF16)
    cnt = 0
    for mt in range(2):
        for q in range(4):
            for j in range(8):
                c = q * 8 + j
                K.op("pe", lambda e, mt=mt, c=c, j=j: e.transpose(out=pm[:, j * 128:(j + 1) * 128],
                                                                    in_=memb[:, mt, c * 128:(c + 1) * 128], identity=ident[:, :]),
                     [membb, identb], [pmb])
            evac(K, _cp(cnt), memT[:, q * 8:(q + 1) * 8, mt * 128:(mt + 1) * 128],
                 pm[:, :].rearrange("p (c t) -> p c t", c=8), [pmb], [memTb])
            cnt += 1
    kvs, kvsb = P.sb("k_kv", [128, 2, 1024], BF16)
    wkvv = wkv_h[:, :].rearrange("(k p) n -> p k n", p=128)
    pkv, pkvb = P.ps("k_pkv", [128, 512], F32)
    wkvs = [P.sb("k_wkv", [128, KC, 512], BF16) for _ in range(2)]
    for half in range(2):
        wk, wkb = wkvs[half]
        for q in range(4):
            K.dma("pool", lambda e, q=q, wk=wk, half=half: e.dma_start(
                out=wk[:, q * 8:(q + 1) * 8, :], in_=wkvv[:, q * 8:(q + 1) * 8, half * 512:(half + 1) * 512]), wkb, writes=[wkb])
        for mt in range(2):
            for k in range(KC):
                K.op("pe", lambda e, k=k, mt=mt, wk=wk: e.matmul(pkv[:, :], lhsT=memT[:, k, mt * 128:(mt + 1) * 128],
                                                                   rhs=wk[:, k, :], start=(k == 0), stop=(k == KC - 1)),
                     [memTb, wkb], [pkvb])
            evac(K, "dve", kvs[:, mt, half * 512:(half + 1) * 512], pkv[:, :], [pkvb], [kvsb])
    kT, kTb = P.sb("k_kT", [128, 4, 256], BF16)
    for h in range(4):
        for mt in range(2):
            K.op("pe", lambda e, h=h, mt=mt: e.transpose(out=pm[:, (h * 2 + mt) * 128:(h * 2 + mt + 1) * 128],
                                                          in_=kvs[:, mt, h * 128:(h + 1) * 128], identity=ident[:, :]),
                 [kvsb, identb], [pmb])
    evac(K, "dve", kT[:, :, :], pm[:, :].rearrange("p (h t) -> p h t", h=4), [pmb], [kTb])
    K.dma("sp", lambda e: e.dma_start(out=mkv.h[:, :], in_=kvs[:, :, :].rearrange("p a b -> p (a b)")), kvsb, reads=[kvsb], writes=[mkv.b[0]])
    K.dma("sp", lambda e: e.dma_start(out=mkt.h[:, :], in_=kT[:, :, :].rearrange("p a b -> p (a b)")), kTb, reads=[kTb], writes=[mkt.b[0]])
    P.end()


def stage_mem(P, tiles, x_src, xT_src, wq_h, wo_h, mkv, mkt, g_ap, b_ap, out_x, out_xb, out_xT, final_out=None):
    K = P.K
    P.begin()
    R = ln_setup(P, g_ap, b_ap, nbuf=2)
    ident, identb = R.ident, R.identb
    scale = 128.0 ** -0.5
    wq, wqb = P.sb("m_wq", [128, KC, 512], BF16)
    wo, wob = P.sb("m_wo", [128, 4, D], BF16)
    wqv = wq_h[:, :].rearrange("(k p) n -> p k n", p=128)
    for q in range(4):
        K.dma("pool", lambda e, q=q: e.dma_start(out=wq[:, q * 8:(q + 1) * 8, :], in_=wqv[:, q * 8:(q + 1) * 8, :]), wqb, writes=[wqb])
    wov = wo_h[:, :].rearrange("(k p) n -> p k n", p=128)
    for q in range(4):
        for hh in range(2):
            K.dma("pool", lambda e, q=q, hh=hh: e.dma_start(out=wo[:, q, hh * 2048:(hh + 1) * 2048],
                                                              in_=wov[:, q, hh * 2048:(hh + 1) * 2048]), wob, writes=[wob])
    kvs, kvsb = P.sb("m_kv", [128, 2, 1024], BF16)
    kT, kTb = P.sb("m_kT", [128, 4, 256], BF16)
    K.dma("sp", lambda e: e.dma_start(out=kvs[:, :, :].rearrange("p a b -> p (a b)"), in_=mkv.h[:, :]), kvsb, reads=[mkv.b[0]], writes=[kvsb])
    K.dma("sp", lambda e: e.dma_start(out=kT[:, :, :].rearrange("p a b -> p (a b)"), in_=mkt.h[:, :]), kTb, reads=[mkt.b[0]], writes=[kTb])
    pm, pmb = P.ps("m_pm", [128, 1024], BF16)
    xts = [P.sb("m_xt", [128, KC, 128], BF16) for _ in range(2)]
    xs = [P.sb("m_x", [128, D], F32) for _ in range(2)]
    qTs = [P.sb("m_qT", [128, 512], BF16) for _ in range(2)]
    pq, pqb = P.ps("m_pq", [128, 512], F32)
    pst, pstb = P.ps("m_ps", [128, 512], F32)
    pss = [(pst[:, 0:256], pstb), (pst[:, 256:512], pstb)]
    pbs = [P.sb("m_p", [128, 256], BF16) for _ in range(2)]
    ptsb = [P.sb("m_pt", [128, 256], BF16) for _ in range(2)]
    sts = [P.sb("m_st", [128, 8], F32) for _ in range(2)]
    po, pob = P.ps("m_po", [128, 128], F32)
    osb, osbb = P.sb("m_o", [128, 512], BF16)
    oT, oTb = P.sb("m_oT", [128, 4, 128], BF16)
    pc = [P.ps("m_pc", [128, 512], F32) for _ in range(2)]
    n2 = 0
    for n, ti in enumerate(tiles):
        xt, xtb = xts[n % 2]
        x, xb_ = xs[n % 2]
        rows = slice(ti * 128, (ti + 1) * 128)
        K.dma("sp", lambda e, xt=xt, ti=ti: e.dma_start(out=xt[:, :, :], in_=xT_src.h[ti, :, :, :]), xtb,
              reads=[xT_src.b[ti]], writes=[xtb])
        K.dma("sp", lambda e, x=x, rows=rows: e.dma_start(out=x[:, :], in_=x_src.h[rows, :]), xb_,
              reads=[x_src.b[ti]], writes=[xb_])
        qT, qTb = qTs[n % 2]
        for h in range(4):
            for k in range(KC):
                K.op("pe", lambda e, h=h, k=k, xt=xt: e.matmul(pq[:, h * 128:(h + 1) * 128], lhsT=wq[:, k, h * 128:(h + 1) * 128],
                                                                 rhs=xt[:, k, :], start=(k == 0), stop=(k == KC - 1)),
                     [wqb, xtb], [pqb])
        K.op("act", lambda e, qT=qT: e.activation(out=qT[:, :], in_=pq[:, :], func=AF.Copy, scale=scale), [pqb], [qTb])
        for h in range(4):
            i2 = n2 % 2
            n2 += 1
            pS, pSb = pss[i2]
            K.op("pe", lambda e, pS=pS, qT=qT, h=h: e.matmul(pS, lhsT=qT[:, h * 128:(h + 1) * 128], rhs=kT[:, h, :],
                                                               start=True, stop=True), [qTb, kTb], [pSb])
            st, stb = sts[i2]
            K.op("dve", lambda e, pS=pS, st=st: e.reduce_max(out=st[:, 1:2], in_=pS, axis=AX.X, negate=True), [pSb], [stb])
            p_, p_b = pbs[i2]
            K.op("act", lambda e, p_=p_, pS=pS, st=st: e.activation(out=p_[:, :], in_=pS, func=AF.Exp, bias=st[:, 1:2],
                                                                      scale=1.0, accum_out=st[:, 2:3]), [pSb, stb], [p_b, stb])
            for j in range(2):
                K.op("pe", lambda e, p_=p_, j=j: e.transpose(out=pm[:, j * 128:(j + 1) * 128], in_=p_[:, j * 128:(j + 1) * 128],
                                                              identity=ident[:, :]), [p_b, identb], [pmb])
            pt_, pt_b = ptsb[i2]
            evac(K, "act", pt_[:, :], pm[:, 0:256], [pmb], [pt_b])
            for j in range(2):
                K.op("pe", lambda e, pt_=pt_, j=j, h=h: e.matmul(po[:, :], lhsT=pt_[:, j * 128:(j + 1) * 128],
                                                                   rhs=kvs[:, j, 512 + h * 128:512 + (h + 1) * 128],
                                                                   start=(j == 0), stop=(j == 1)), [pt_b, kvsb], [pob])
            K.op("dve", lambda e, st=st: e.reciprocal(out=st[:, 3:4], in_=st[:, 2:3]), [stb], [stb])
            K.op("dve", lambda e, st=st, h=h: e.tensor_scalar_mul(out=osb[:, h * 128:(h + 1) * 128], in0=po[:, :], scalar1=st[:, 3:4]),
                 [pob, stb], [osbb])
        for h in range(4):
            K.op("pe", lambda e, h=h: e.transpose(out=pm[:, h * 128:(h + 1) * 128], in_=osb[:, h * 128:(h + 1) * 128],
                                                   identity=ident[:, :]), [osbb, identb], [pmb])
        evac(K, "dve", oT[:, :, :], pm[:, 0:512].rearrange("p (h t) -> p h t", h=4), [pmb], [oTb])
        for cb in range(8):
            pcc, pccb = pc[cb % 2]
            for j in range(4):
                K.op("pe", lambda e, pcc=pcc, j=j, cb=cb: e.matmul(pcc[:, :], lhsT=oT[:, j, :], rhs=wo[:, j, cb * 512:(cb + 1) * 512],
                                                                     start=(j == 0), stop=(j == 3)), [oTb, wob], [pccb])
            K.op("dve", lambda e, pcc=pcc, x=x, cb=cb: e.scalar_tensor_tensor(
                out=x[:, cb * 512:(cb + 1) * 512], in0=x[:, cb * 512:(cb + 1) * 512], scalar=ALPHA, in1=pcc[:, :],
                op0=ALU.mult, op1=ALU.add), [pccb, xb_], [xb_])
        fo = None
        if final_out is not None and ti >= P.geo.own0:
            fo = (final_out, (ti - P.geo.own0) * 128)
        ln_tile(P, R, x, xb_, ti, out_x=out_x, out_xb=out_xb, out_xT=out_xT, final_out=fo)
    P.end()


def stage_router(P, tiles, xT_src, wr_h, br_h, srec, tslot, ztok):
    K = P.K
    P.begin()
    nt = len(tiles)
    wr, wrb = P.sb("r_w", [128, KC, 36], BF16)
    K.dma("pool", lambda e: e.dma_start(out=wr[:, :, :], in_=wr_h[:, :].rearrange("(k p) n -> p k n", p=128)), wrb, writes=[wrb])
    br, brb = P.sb("r_b", [128, 36], F32)
    K.dma("sp", lambda e: e.dma_start(out=br[:, :], in_=br_h[0:1, :].partition_broadcast(128)), brb, writes=[brb])
    tri, trib = P.sb("r_tri", [128, 128], BF16)
    K.dma("pool", lambda e: e.dma_start(out=tri[:, :], in_=P.ins["cst_tri"][:, :]), trib, writes=[trib])
    ones, onesb = P.sb("r_ones", [128, 128], BF16)
    K.op("pool", lambda e: e.memset(ones[:, :], 1.0), [], [onesb])
    iot, iotb = P.sb("r_iota", [128, 32], F32)
    K.dma("sp", lambda e: e.dma_start(out=iot[:, :], in_=P.ins["cst_iota"][:, :]), iotb, writes=[iotb])
    tokid, tokidb = P.sb("r_tokid", [128, P.geo.nt_all], I32)
    K.dma("sp", lambda e: e.dma_start(out=tokid[:, :], in_=P.ins["cst_tokid"][:, :]), tokidb, writes=[tokidb])
    init, initb = P.sb("r_init", [128, NSLOT // 128 + 1, 2], I32)
    K.op("pool", lambda e: e.memset(init[:, :, :], 0), [], [initb])
    K.op("pool", lambda e: e.memset(init[:, :, 0:1], ztok), [initb], [initb])
    initdone = P.newbuf("r_initdone")
    K.dma("sp", lambda e: e.dma_start(out=srec.h[:, :].rearrange("(p n) c -> p n c", p=128), in_=init[:, :, :]), initb,
          reads=[initb], writes=[srec.b[0], initdone])
    A_all, A_allb = P.sb("r_A", [128, nt, 32], BF16, nbuf=nt)
    xts = [P.sb("r_xt", [128, KC, 128], BF16) for _ in range(2)]
    pl, plb = P.ps("r_pl", [128, 36], F32)
    pr, prb = P.ps("r_pr", [128, 32], F32)
    W = [P.sb("r_work", [128, 256], F32) for _ in range(2)]
    recs = [P.sb("r_rec", [128, 2, 2], I32) for _ in range(2)]
    sls = [P.sb("r_sl", [128, 2], I32) for _ in range(2)]
    for n, ti in enumerate(tiles):
        xt, xtb = xts[n % 2]
        K.dma("sp", lambda e, xt=xt, ti=ti: e.dma_start(out=xt[:, :, :], in_=xT_src.h[ti, :, :, :]), xtb,
              reads=[xT_src.b[ti]], writes=[xtb])
        for k in range(KC):
            K.op("pe", lambda e, xt=xt, k=k: e.matmul(pl[:, :], lhsT=xt[:, k, :], rhs=wr[:, k, :], start=(k == 0), stop=(k == KC - 1)),
                 [xtb, wrb], [plb])
        w, wb_ = W[n % 2]
        L = w[:, 0:36]
        dv = lambda fn, rd=(), wr_=None: K.op("dve", fn, [wb_] + list(rd), [wb_] if wr_ is None else wr_)
        K.op("dve", lambda e, w=w: e.tensor_tensor(out=w[:, 0:36], in0=pl[:, :], in1=br[:, :], op=ALU.add), [plb, brb], [wb_])
        dv(lambda e, w=w: e.reduce_max(out=w[:, 44:45], in_=w[:, 0:4], axis=AX.X))
        dv(lambda e, w=w: e.tensor_scalar(out=w[:, 36:40], in0=w[:, 0:4], scalar1=w[:, 44:45], scalar2=None, op0=ALU.is_equal))
        dv(lambda e, w=w: e.tensor_scalar_mul(out=w[:, 45:46], in0=w[:, 44:45], scalar1=-1.0))
        K.op("act", lambda e, w=w: e.activation(out=w[:, 40:44], in_=w[:, 0:4], func=AF.Exp, bias=w[:, 45:46], scale=1.0,
                                                accum_out=w[:, 46:47]), [wb_], [wb_])
        dv(lambda e, w=w: e.reciprocal(out=w[:, 47:48], in_=w[:, 46:47]))
        dv(lambda e, w=w: e.tensor_scalar_mul(out=w[:, 48:56], in0=w[:, 4:12], scalar1=w[:, 36:37]))
        for g in range(1, 4):
            dv(lambda e, w=w, g=g: e.scalar_tensor_tensor(out=w[:, 48:56], in0=w[:, 4 + g * 8:12 + g * 8], scalar=w[:, 36 + g:37 + g],
                                                          in1=w[:, 48:56], op0=ALU.mult, op1=ALU.add))
        dv(lambda e, w=w: e.max(out=w[:, 56:64], in_=w[:, 48:56]))
        dv(lambda e, w=w: e.tensor_scalar(out=w[:, 64:72], in0=w[:, 48:56], scalar1=w[:, 56:57], scalar2=None, op0=ALU.is_equal))
        dv(lambda e, w=w: e.tensor_scalar(out=w[:, 72:80], in0=w[:, 48:56], scalar1=w[:, 57:58], scalar2=None, op0=ALU.is_equal))
        dv(lambda e, w=w: e.tensor_tensor(out=w[:, 208:209], in0=w[:, 57:58], in1=w[:, 56:57], op=ALU.subtract))
        K.op("act", lambda e, w=w: e.activation(out=w[:, 209:210], in_=w[:, 208:209], func=AF.Exp), [wb_], [wb_])
        dv(lambda e, w=w: e.tensor_scalar_add(out=w[:, 210:211], in0=w[:, 209:210], scalar1=1.0))
        dv(lambda e, w=w: e.reciprocal(out=w[:, 211:212], in_=w[:, 210:211]))
        dv(lambda e, w=w: e.tensor_tensor(out=w[:, 212:213], in0=w[:, 211:212], in1=w[:, 47:48], op=ALU.mult))
        dv(lambda e, w=w: e.tensor_tensor(out=w[:, 213:214], in0=w[:, 47:48], in1=w[:, 212:213], op=ALU.subtract))
        for g in range(4):
            dv(lambda e, w=w, g=g: e.tensor_scalar_mul(out=w[:, 80 + g * 8:88 + g * 8], in0=w[:, 64:72], scalar1=w[:, 36 + g:37 + g]))
            dv(lambda e, w=w, g=g: e.tensor_scalar_mul(out=w[:, 112 + g * 8:120 + g * 8], in0=w[:, 72:80], scalar1=w[:, 36 + g:37 + g]))
        K.op("dve", lambda e, w=w, n=n: e.tensor_tensor(out=A_all[:, n, :], in0=w[:, 80:112], in1=w[:, 112:144], op=ALU.add),
             [wb_], [A_allb[n]])
        K.op("pe", lambda e, n=n: e.matmul(pr[:, :], lhsT=tri[:, :], rhs=A_all[:, n, :], start=True, stop=(n == 0)),
             [trib, A_allb[n]], [prb])
        for m in range(n):
            K.op("pe", lambda e, m=m, n=n: e.matmul(pr[:, :], lhsT=ones[:, :], rhs=A_all[:, m, :], start=False, stop=(m == n - 1)),
                 [onesb, A_allb[m]], [prb])
        K.op("dve", lambda e, w=w: e.tensor_copy(out=w[:, 176:208], in_=pr[:, :]), [prb], [wb_])
        K.op("dve", lambda e, w=w: e.tensor_tensor(out=w[:, 144:176], in0=w[:, 176:208], in1=iot[:, :], op=ALU.add), [wb_, iotb], [wb_])
        for kk in range(2):
            a0 = 80 + kk * 32
            dv(lambda e, w=w, a0=a0: e.tensor_tensor(out=w[:, 216:248], in0=w[:, a0:a0 + 32], in1=w[:, 176:208], op=ALU.mult))
            dv(lambda e, w=w, kk=kk: e.reduce_sum(out=w[:, 248 + kk:249 + kk], in_=w[:, 216:248], axis=AX.X))
            dv(lambda e, w=w, a0=a0: e.tensor_tensor(out=w[:, 216:248], in0=w[:, a0:a0 + 32], in1=w[:, 144:176], op=ALU.mult))
            dv(lambda e, w=w, kk=kk: e.reduce_sum(out=w[:, 250 + kk:251 + kk], in_=w[:, 216:248], axis=AX.X))
            dv(lambda e, w=w, kk=kk: e.tensor_scalar(out=w[:, 252 + kk:253 + kk], in0=w[:, 248 + kk:249 + kk], scalar1=float(CAP) - 0.5,
                                                     scalar2=None, op0=ALU.is_ge))
            dv(lambda e, w=w, kk=kk: e.tensor_scalar(out=w[:, 254 + kk:255 + kk], in0=w[:, 250 + kk:251 + kk], scalar1=-1.0,
                                                     scalar2=float(NSLOT), op0=ALU.mult, op1=ALU.add))
            dv(lambda e, w=w, kk=kk: e.tensor_tensor(out=w[:, 254 + kk:255 + kk], in0=w[:, 254 + kk:255 + kk],
                                                     in1=w[:, 252 + kk:253 + kk], op=ALU.mult))
            dv(lambda e, w=w, kk=kk: e.tensor_tensor(out=w[:, 250 + kk:251 + kk], in0=w[:, 250 + kk:251 + kk],
                                                     in1=w[:, 254 + kk:255 + kk], op=ALU.add))
        sl, slb = sls[n % 2]
        K.op("dve", lambda e, w=w, sl=sl: e.tensor_copy(out=sl[:, :], in_=w[:, 250:252]), [wb_], [slb])
        rec, recb = recs[n % 2]
        for kk in range(2):
            K.op("dve", lambda e, rec=rec, kk=kk, ti=ti: e.tensor_copy(out=rec[:, kk, 0:1], in_=tokid[:, ti:ti + 1]), [tokidb], [recb])
            K.op("dve", lambda e, rec=rec, kk=kk, w=w: e.tensor_copy(out=rec[:, kk, 1:2].bitcast(F32), in_=w[:, 212 + kk:213 + kk]),
                 [wb_], [recb])
        K.dma("sp", lambda e, sl=sl, ti=ti: e.dma_start(out=tslot.h[ti * 128:(ti + 1) * 128, :], in_=sl[:, :]), slb,
              reads=[slb], writes=[tslot.b[ti]])
        for kk in range(2):
            K.dma("pool", lambda e, rec=rec, sl=sl, kk=kk: e.indirect_dma_start(
                out=srec.h[:, :], out_offset=bass.IndirectOffsetOnAxis(ap=sl[:, kk:kk + 1], axis=0),
                in_=rec[:, kk, :], in_offset=None), recb, reads=[recb, slb, initdone], writes=[srec.b[0]])
    P.end()


def stage_experts(P, xb_src, srec, wg_h, wu_h, wd_h, ys):
    K = P.K
    P.begin()
    ident, identb = load_ident(P)
    wg_t = [P.sb("e_wg", [128, KC, DEXP], BF16, nbuf=4) for _ in range(2)]
    wu_t = [P.sb("e_wu", [128, KC, DEXP], BF16, nbuf=4) for _ in range(2)]
    wd_t = [P.sb("e_wd", [128, 3, D], BF16, nbuf=6) for _ in range(2)]
    NSTG = 2
    stg = [P.sb("e_stg", [128, 3072], F32) for _ in range(NSTG)]
    xes = [P.sb("e_xe", [128, D], BF16) for _ in range(1)]
    xeTs = [P.sb("e_xeT", [128, KC, 128], BF16) for _ in range(1)]
    recs = [P.sb("e_rec", [128, 2], I32) for _ in range(2)]
    sgs = [P.sb("e_sg", [128, DEXP], F32) for _ in range(2)]
    us = [P.sb("e_u", [128, DEXP], BF16) for _ in range(2)]
    uTs = [P.sb("e_uT", [128, 3, 128], BF16) for _ in range(2)]
    NY = 3
    yss = [P.sb("e_y", [128, 512], F32) for _ in range(NY)]
    pts = [P.ps("e_pt", [128, 1024], BF16) for _ in range(2)]
    ph1 = P.ps("e_ph1", [128, 512], F32)
    ph2 = P.ps("e_ph2", [128, 512], F32)
    pys = [P.ps("e_py", [128, 512], F32) for _ in range(2)]
    z, zb = yss[0]
    K.op("pool", lambda e: e.memset(z[:, :], 0.0), [], [zb])
    for cb in range(8):
        K.dma("pool", lambda e, cb=cb: e.dma_start(out=ys.h[NSLOT:NSLOT + 128, cb * 512:(cb + 1) * 512], in_=z[:, :]), zb,
              reads=[zb], writes=[ys.b[0]])
    for x_, xb__ in xes:
        K.op("pool", lambda e, x_=x_: e.memset(x_[:, :], 0.0), [], [xb__])
    ceng = ["act", "dve"]
    state = dict(ni=0, nc=0)
    pend = []

    def pieces_of(ex):
        out = []
        slot = ex % 2
        for src, (dst, dbufs) in ((wg_h, wg_t[slot]), (wu_h, wu_t[slot])):
            v = src[ex, :, :].rearrange("(k p) n -> p k n", p=128)
            for q in range(4):
                out.append((v[:, q * 8:(q + 1) * 8, :], 8, dst[:, q * 8:(q + 1) * 8, :], dbufs[q], ex - 2))
        v = wd_h[ex, :, :].rearrange("(k p) n -> p k n", p=128)
        wd, wdbs = wd_t[slot]
        for k in range(3):
            for hh in range(2):
                out.append((v[:, k, hh * 2048:(hh + 1) * 2048], None, wd[:, k, hh * 2048:(hh + 1) * 2048], wdbs[k * 2 + hh], ex - 2))
        return out

    allp = []
    for ex in range(NEXP):
        allp.extend(pieces_of(ex))

    def issue():
        while state["ni"] < len(allp) and len(pend) < NSTG:
            src_ap, ksplit, dst_ap, dst_buf, rdy = allp[state["ni"]]
            st, stb = stg[state["ni"] % NSTG]
            sv = st[:, 0:2048] if ksplit is None else st[:, :].rearrange("p (k n) -> p k n", k=ksplit)
            K.dma("sp", lambda e, sv=sv, src_ap=src_ap: e.dma_start(out=sv, in_=src_ap), stb, writes=[stb])
            pend.append((sv, stb, dst_ap, dst_buf, rdy))
            state["ni"] += 1

    def step(done_ex):
        issue()
        if pend and pend[0][4] <= done_ex:
            sv, stb, dst_ap, dst_buf, rdy = pend.pop(0)
            evac(K, ceng[state["nc"] % 2], dst_ap, sv, [stb], [dst_buf])
            state["nc"] += 1
            issue()
            return True
        return False

    def fetch(bi):
        s0_ = bi * 128
        rec_, recb_ = recs[bi % 2]
        K.dma("pool", lambda e: e.dma_start(out=rec_[:, :], in_=srec.h[s0_:s0_ + 128, :]), recb_,
              reads=[srec.b[0]], writes=[recb_])
        xe_, xeb_ = xes[0]
        K.dma("pool", lambda e: e.indirect_dma_start(
            out=xe_[:, :], out_offset=None, in_=xb_src.h[:, :],
            in_offset=bass.IndirectOffsetOnAxis(ap=rec_[:, 0:1], axis=0)), xeb_,
            reads=[recb_] + xb_src.b, writes=[xeb_])

    for _ in range(14):
        step(-1)
    nb = 0
    ny = 0
    for ex in range(NEXP):
        wg, wgbs = wg_t[ex % 2]
        wu, wubs = wu_t[ex % 2]
        wd, wdbs = wd_t[ex % 2]
        for c in range(CAPB):
            for _ in range(3):
                step(ex - 1)
            s0 = ex * CAP + c * 128
            i2 = nb % 2
            if nb == 0:
                fetch(0)
            nb += 1
            rec, recb = recs[i2]
            xe, xeb = xes[0]
            xeT, xeTb = xeTs[0]
            for q in range(4):
                pt, ptb = pts[q % 2]
                for j in range(8):
                    cc = q * 8 + j
                    K.op("pe", lambda e, pt=pt, xe=xe, cc=cc, j=j: e.transpose(out=pt[:, j * 128:(j + 1) * 128],
                                                                               in_=xe[:, cc * 128:(cc + 1) * 128], identity=ident[:, :]),
                         [xeb, identb], [ptb])
                evac(K, _cp(q), xeT[:, q * 8:(q + 1) * 8, :], pt[:, :].rearrange("p (c t) -> p c t", c=8), [ptb], [xeTb])
            if nb < NEXP * CAPB:
                fetch(nb)
            for (ph, phb), (wt, wtbs) in ((ph1, (wg, wgbs)), (ph2, (wu, wubs))):
                for k in range(KC):
                    K.op("pe", lambda e, ph=ph, xeT=xeT, wt=wt, k=k: e.matmul(ph[:, 0:DEXP], lhsT=xeT[:, k, :], rhs=wt[:, k, :],
                                                                              start=(k == 0), stop=(k == KC - 1)),
                         [xeTb, wtbs[k // 8]], [phb])
            sg, sgb = sgs[i2]
            K.op("act", lambda e, sg=sg: e.activation(out=sg[:, :], in_=ph1[0][:, 0:DEXP], func=AF.Silu), [ph1[1]], [sgb])
            u, ub = us[i2]
            K.op("dve", lambda e, u=u, sg=sg, rec=rec: e.scalar_tensor_tensor(
                out=u[:, :], in0=ph2[0][:, 0:DEXP], scalar=rec[:, 1:2].bitcast(F32), in1=sg[:, :], op0=ALU.mult, op1=ALU.mult),
                [ph2[1], recb, sgb], [ub])
            uT, uTb = uTs[i2]
            pt, ptb = pts[0]
            for j in range(3):
                K.op("pe", lambda e, pt=pt, u=u, j=j: e.transpose(out=pt[:, j * 128:(j + 1) * 128], in_=u[:, j * 128:(j + 1) * 128],
                                                                  identity=ident[:, :]), [ub, identb], [ptb])
            evac(K, "act", uT[:, :, :], pt[:, 0:384].rearrange("p (c t) -> p c t", c=3), [ptb], [uTb])
            for cb in range(8):
                py, pyb = pys[cb % 2]
                for j in range(3):
                    K.op("pe", lambda e, py=py, uT=uT, j=j, cb=cb, wd=wd: e.matmul(py[:, :], lhsT=uT[:, j, :], rhs=wd[:, j, cb * 512:(cb + 1) * 512],
                                                                                   start=(j == 0), stop=(j == 2)), [uTb, wdbs[j * 2 + cb // 4]], [pyb])
                y, yb_ = yss[ny % NY]
                evac(K, _cp(ny), y[:, :], py[:, :], [pyb], [yb_])
                K.dma("pool", lambda e, y=y, s0=s0, cb=cb: e.dma_start(out=ys.h[s0:s0 + 128, cb * 512:(cb + 1) * 512], in_=y[:, :]), yb_,
                      reads=[yb_], writes=[ys.b[0]])
                ny += 1
                if cb % 4 == 3:
                    step(ex - 1)
    P.end()


def stage_combine(P, tiles, x_src, ys, tslot, g_ap, b_ap, out_x, out_xb, out_xT, final_out=None):
    K = P.K
    P.begin()
    R = ln_setup(P, g_ap, b_ap, nbuf=2)
    xs = [P.sb("c_x", [128, D], F32) for _ in range(2)]
    y1s = [P.sb("c_y1", [128, D], F32) for _ in range(2)]
    y2s = [P.sb("c_y2", [128, D], F32) for _ in range(2)]
    sls = [P.sb("c_sl", [128, 2], I32) for _ in range(2)]
    for n, ti in enumerate(tiles):
        x, xb_ = xs[n % 2]
        y1, y1b = y1s[n % 2]
        y2, y2b = y2s[n % 2]
        sl, slb = sls[n % 2]
        rows = slice(ti * 128, (ti + 1) * 128)
        K.dma("sp", lambda e, sl=sl, rows=rows: e.dma_start(out=sl[:, :], in_=tslot.h[rows, :]), slb, reads=[tslot.b[ti]], writes=[slb])
        K.dma("sp", lambda e, x=x, rows=rows: e.dma_start(out=x[:, :], in_=x_src.h[rows, :]), xb_, reads=[x_src.b[ti]], writes=[xb_])
        for yy, yyb, kk in ((y1, y1b, 0), (y2, y2b, 1)):
            K.dma("pool", lambda e, yy=yy, sl=sl, kk=kk: e.indirect_dma_start(
                out=yy[:, :], out_offset=None, in_=ys.h[:, :], in_offset=bass.IndirectOffsetOnAxis(ap=sl[:, kk:kk + 1], axis=0)),
                yyb, reads=[slb, ys.b[0]], writes=[yyb])
        K.op("dve", lambda e, x=x, y1=y1: e.scalar_tensor_tensor(out=x[:, :], in0=x[:, :], scalar=ALPHA, in1=y1[:, :],
                                                                 op0=ALU.mult, op1=ALU.add), [xb_, y1b], [xb_])
        K.op("pool", lambda e, x=x, y2=y2: e.tensor_tensor(out=x[:, :], in0=x[:, :], in1=y2[:, :], op=ALU.add), [xb_, y2b], [xb_])
        fo = None
        if final_out is not None and ti >= P.geo.own0:
            fo = (final_out, (ti - P.geo.own0) * 128)
        ln_tile(P, R, x, xb_, ti, out_x=out_x, out_xb=out_xb, out_xT=out_xT, final_out=fo)
    P.end()


PADC = 32


def stage_glu(P, tiles, xT_src, win_h, bin_h, ut):
    K = P.K
    geo = P.geo
    P.begin()
    NTG = 8
    CBW = 256
    XS, xsb = P.sb("u_xs", [128, KC, NTG * 128], BF16, nbuf=NTG)
    was = [P.sb("u_wa", [128, KC, CBW], BF16) for _ in range(2)]
    wgs = [P.sb("u_wg", [128, KC, CBW], BF16) for _ in range(2)]
    bin_, binb = P.sb("u_bin", [128, 64], F32)
    K.dma("sp", lambda e: e.dma_start(out=bin_[:, :], in_=bin_h[:, :]), binb, writes=[binb])
    tv, tvb = P.sb("u_tv", [128, geo.nt_all * 128], F32)
    K.dma("sp", lambda e: e.dma_start(out=tv[:, :], in_=P.ins["tokvalid"][0:1, :].partition_broadcast(128)), tvb, writes=[tvb])
    z, zb = P.sb("u_z", [128, 32, PADC], BF16)
    K.op("pool", lambda e: e.memset(z[:, :, :], 0.0), [], [zb])
    K.dma("sp", lambda e: e.dma_start(out=ut.h[:, :, 0:PADC].rearrange("c p t -> p c t"), in_=z[:, :, :]), zb, reads=[zb], writes=ut.b)
    sgs = [P.sb("u_sg", [128, 512], F32) for _ in range(2)]
    uss = [P.sb("u_u", [128, 512], BF16) for _ in range(3)]
    pas = [P.ps("u_pa", [128, 512], F32) for _ in range(2)]
    pgs = [P.ps("u_pg", [128, 512], F32) for _ in range(2)]
    wv = win_h[:, :].rearrange("(k p) n -> p k n", p=128)
    t0 = tiles[0]
    groups = [tiles[i:i + NTG] for i in range(0, len(tiles), NTG)]
    nblk = 0
    nn = 0

    def load_w(cb, slot):
        for (w, wb_), off in ((was[slot], 0), (wgs[slot], D)):
            for q in range(4):
                K.dma("pool", lambda e, w=w, q=q, off=off, cb=cb: e.dma_start(
                    out=w[:, q * 8:(q + 1) * 8, :], in_=wv[:, q * 8:(q + 1) * 8, off + cb * CBW:off + (cb + 1) * CBW]), wb_, writes=[wb_])

    ncb = D // CBW
    for grp in groups:
        for s, ti in enumerate(grp):
            K.dma("sp", lambda e, s=s, ti=ti: e.dma_start(out=XS[:, :, s * 128:(s + 1) * 128], in_=xT_src.h[ti, :, :, :]),
                  xsb[s], reads=[xT_src.b[ti]], writes=[xsb[s]])
        load_w(0, nblk % 2)
        for cb in range(ncb):
            if cb + 1 < ncb:
                load_w(cb + 1, (nblk + 1) % 2)
            wa, wab = was[nblk % 2]
            wg, wgb = wgs[nblk % 2]
            for m in range(CBW // 128):
                ch = cb * (CBW // 128) + m
                for tg in range(0, len(grp), 4):
                    ntk = min(4, len(grp) - tg)
                    ncol = ntk * 128
                    pa, pab = pas[nn % 2]
                    pg, pgb = pgs[nn % 2]
                    rd = [xsb[tg + j] for j in range(ntk)]
                    for (pp, ppb), (w, wb_) in (((pa, pab), (wa, wab)), ((pg, pgb), (wg, wgb))):
                        for k in range(KC):
                            K.op("pe", lambda e, pp=pp, w=w, k=k, m=m, tg=tg, ncol=ncol: e.matmul(
                                pp[:, 0:ncol], lhsT=w[:, k, m * 128:(m + 1) * 128], rhs=XS[:, k, tg * 128:tg * 128 + ncol],
                                start=(k == 0), stop=(k == KC - 1)), rd + [wb_], [ppb])
                    sg, sgb = sgs[nn % 2]
                    u, ub = uss[nn % 3]
                    K.op("act", lambda e, sg=sg, pg=pg, ch=ch, ncol=ncol: e.activation(
                        out=sg[:, 0:ncol], in_=pg[:, 0:ncol], func=AF.Sigmoid, bias=bin_[:, 32 + ch:33 + ch], scale=1.0), [pgb, binb], [sgb])
                    K.op("dve", lambda e, u=u, pa=pa, sg=sg, ch=ch, ncol=ncol: e.scalar_tensor_tensor(
                        out=u[:, 0:ncol], in0=pa[:, 0:ncol], scalar=bin_[:, ch:ch + 1], in1=sg[:, 0:ncol], op0=ALU.add, op1=ALU.mult),
                        [pab, binb, sgb], [ub])
                    tfirst = grp[tg]
                    if tfirst < geo.own0:
                        K.op("pool", lambda e, u=u, tfirst=tfirst, ncol=ncol: e.tensor_tensor(
                            out=u[:, 0:ncol], in0=u[:, 0:ncol], in1=tv[:, tfirst * 128:tfirst * 128 + ncol], op=ALU.mult), [ub, tvb], [ub])
                    c0 = PADC + (tfirst - t0) * 128
                    K.dma("sp", lambda e, u=u, ch=ch, c0=c0, ncol=ncol: e.dma_start(out=ut.h[ch, :, c0:c0 + ncol], in_=u[:, 0:ncol]),
                          ub, reads=[ub], writes=[ut.b[ch]])
                    nn += 1
            nblk += 1
    P.end()


def stage_dwconv(P, tiles, ut, wdw_h, bdw_h, f_dst):
    K = P.K
    P.begin()
    nt = len(tiles)
    Tp = nt * 128
    identb16, identb16b = load_ident(P)
    identf, identfb = P.sb("d_identf", [128, 128], F32)
    K.dma("sp", lambda e: e.dma_start(out=identf[:, :], in_=P.ins["cst_ident"][:, :]), identfb, writes=[identfb])
    wdw, wdwb = P.sb("d_wdw", [128, 32, 31], F32)
    K.dma("sp", lambda e: e.dma_start(out=wdw[:, :, :], in_=wdw_h[:, :, :]), wdwb, writes=[wdwb])
    bdw, bdwb = P.sb("d_bdw", [128, 32], F32)
    K.dma("sp", lambda e: e.dma_start(out=bdw[:, :], in_=bdw_h[:, :]), bdwb, writes=[bdwb])
    uts = [P.sb("d_ut", [128, PADC + Tp], BF16) for _ in range(2)]
    dgs = [P.sb("d_dg", [128, 31, 128], BF16) for _ in range(2)]
    cvs = [P.sb("d_cv", [128, 512], F32) for _ in range(2)]
    ots = [P.sb("d_ot", [128, 4, 128], F32) for _ in range(2)]
    pcs = [P.ps("d_pc", [128, 512], F32) for _ in range(2)]
    pts = [P.ps("d_pt", [128, 512], F32) for _ in range(2)]
    nn = 0

    def load_ut(ch, slot):
        u, ub = uts[slot]
        K.dma("sp", lambda e, u=u, ch=ch: e.dma_start(out=u[:, :], in_=ut.h[ch, :, 0:PADC + Tp]), ub, reads=[ut.b[ch]], writes=[ub])

    load_ut(0, 0)
    for ch in range(32):
        if ch + 1 < 32:
            load_ut(ch + 1, (ch + 1) % 2)
        u, ub = uts[ch % 2]
        dg, dgb = dgs[ch % 2]
        for j in range(31):
            K.op("dve" if j % 2 == 0 else "pool", lambda e, dg=dg, j=j, ch=ch: e.tensor_scalar_mul(
                out=dg[:, j, :], in0=identb16[:, :], scalar1=wdw[:, ch, j:j + 1]), [identb16b, wdwb], [dgb])
        for tg in range(0, nt, 4):
            ntk = min(4, nt - tg)
            ncol = ntk * 128
            pc, pcb = pcs[nn % 2]
            for j in range(31):
                c0 = PADC + tg * 128 - 30 + j
                K.op("pe", lambda e, pc=pc, dg=dg, u=u, j=j, c0=c0, ncol=ncol: e.matmul(
                    pc[:, 0:ncol], lhsT=dg[:, j, :], rhs=u[:, c0:c0 + ncol], start=(j == 0), stop=(j == 30)), [dgb, ub], [pcb])
            cv, cvb = cvs[nn % 2]
            K.op("act", lambda e, cv=cv, pc=pc, ch=ch, ncol=ncol: e.activation(out=cv[:, 0:ncol], in_=pc[:, 0:ncol], func=AF.Identity,
                                                                               bias=bdw[:, ch:ch + 1], scale=1.0), [pcb, bdwb], [cvb])
            pt, ptb = pts[nn % 2]
            for j in range(ntk):
                K.op("pe", lambda e, pt=pt, cv=cv, j=j: e.transpose(out=pt[:, j * 128:(j + 1) * 128], in_=cv[:, j * 128:(j + 1) * 128],
                                                                    identity=identf[:, :]), [cvb, identfb], [ptb])
            ot, otb = ots[nn % 2]
            evac(K, "dve", ot[:, 0:ntk, :], pt[:, 0:ncol].rearrange("p (n c) -> p n c", n=ntk), [ptb], [otb])
            r0 = tiles[tg] * 128
            K.dma("sp", lambda e, ot=ot, r0=r0, ntk=ntk, ch=ch: e.dma_start(
                out=f_dst.h[r0:r0 + ntk * 128, ch * 128:(ch + 1) * 128].rearrange("(n p) c -> p n c", p=128), in_=ot[:, 0:ntk, :]),
                otb, reads=[otb], writes=[f_dst.b[tiles[tg + j]] for j in range(ntk)])
            nn += 1
    P.end()


def stage_pool(P, tiles, xb_src, wp_h, scale_ap, f_dst):
    K = P.K
    geo = P.geo
    P.begin()
    wp, wpb = P.sb("p_wp", [128, 4, 8, 1024], BF16)
    for g in range(4):
        v = wp_h[g, :, :].rearrange("(k p) n -> p k n", p=128)
        for q in range(2):
            K.dma("pool", lambda e, g=g, q=q, v=v: e.dma_start(out=wp[:, g, q * 4:(q + 1) * 4, :], in_=v[:, q * 4:(q + 1) * 4, :]), wpb, writes=[wpb])
    sc, scb = P.sb("p_sc", [128, D], F32)
    K.dma("sp", lambda e: e.dma_start(out=sc[:, :], in_=scale_ap.partition_broadcast(128)), scb, writes=[scb])
    Bg, Bgb = P.sb("p_B", [128, 8, 128], BF16)
    B0, B0b = P.sb("p_B0", [128, 8, 128], BF16)
    K.dma("pool", lambda e: e.dma_start(out=Bg[:, :, :], in_=P.ins["cst_poolB"][:, :, :].rearrange("n p t -> p n t")), Bgb, writes=[Bgb])
    K.dma("pool", lambda e: e.dma_start(out=B0[:, :, :], in_=P.ins["poolB0"][:, :, :].rearrange("n p t -> p n t")), B0b, writes=[B0b])
    xbs = [P.sb("p_xb", [128, D], BF16) for _ in range(3)]
    pTs = [P.sb("p_pT", [128, KC, 128], BF16) for _ in range(2)]
    fss = [P.sb("p_f", [128, 512], F32) for _ in range(4)]
    pps = [P.ps("p_pp", [128, 512], F32) for _ in range(2)]
    pys = [P.ps("p_py", [128, 512], F32) for _ in range(2)]
    nn = 0
    nf = 0
    prev = None
    for n, ti in enumerate(tiles):
        xb, xbb = xbs[n % 3]
        rows = slice(ti * 128, (ti + 1) * 128)
        K.dma("sp", lambda e, xb=xb, rows=rows: e.dma_start(out=xb[:, :], in_=xb_src.h[rows, :]), xbb, reads=[xb_src.b[ti]], writes=[xbb])
        Bm, Bmb = (B0, B0b) if ti == geo.own0 else (Bg, Bgb)
        pT, pTb = pTs[n % 2]
        for q in range(8):
            pp, ppb = pps[nn % 2]
            nn += 1
            for j in range(4):
                c = q * 4 + j
                g = c // 8
                K.op("pe", lambda e, pp=pp, xb=xb, c=c, j=j, g=g, Bm=Bm: e.matmul(
                    pp[:, j * 128:(j + 1) * 128], lhsT=xb[:, c * 128:(c + 1) * 128], rhs=Bm[:, g * 2, :], start=True, stop=(prev is None)),
                    [xbb, Bmb], [ppb])
                if prev is not None:
                    pxb, pxbb = prev
                    K.op("pe", lambda e, pp=pp, pxb=pxb, c=c, j=j, g=g, Bm=Bm: e.matmul(
                        pp[:, j * 128:(j + 1) * 128], lhsT=pxb[:, c * 128:(c + 1) * 128], rhs=Bm[:, g * 2 + 1, :], start=False, stop=True),
                        [pxbb, Bmb], [ppb])
            evac(K, _cp(q), pT[:, q * 4:(q + 1) * 4, :], pp[:, :].rearrange("p (c t) -> p c t", c=4), [ppb], [pTb])
        for g in range(4):
            for cb in range(2):
                py, pyb = pys[nf % 2]
                for k in range(8):
                    K.op("pe", lambda e, py=py, pT=pT, g=g, k=k, cb=cb: e.matmul(
                        py[:, :], lhsT=pT[:, g * 8 + k, :], rhs=wp[:, g, k, cb * 512:(cb + 1) * 512], start=(k == 0), stop=(k == 7)),
                        [pTb, wpb], [pyb])
                f, fb = fss[nf % 4]
                col = g * 1024 + cb * 512
                K.op("dve", lambda e, f=f, py=py, col=col: e.tensor_tensor(out=f[:, :], in0=py[:, :], in1=sc[:, col:col + 512], op=ALU.mult),
                     [pyb, scb], [fb])
                K.dma("sp", lambda e, f=f, rows=rows, col=col: e.dma_start(out=f_dst.h[rows, col:col + 512], in_=f[:, :]), fb,
                      reads=[fb], writes=[f_dst.b[ti]])
                nf += 1
        prev = (xb, xbb)
    P.end()


def build(geo):
    P = Prog(geo)
    nc = P.nc
    K = P.K
    NT = geo.nt_all
    T = NT * 128
    layers = geo.layers
    x_in = DramT(nc, "x_ext", [T, D], F32, NT, kind="ExternalInput")
    P.ins["x_ext"] = x_in.h
    P.inp("mem", [256, D])
    P.inp("kmask", [1, T])
    P.inp("tokvalid", [1, T])
    P.inp("poolB0", [8, 128, 128])
    P.inp("cst_ident", [128, 128])
    P.inp("cst_tri", [128, 128])
    P.inp("cst_iota", [128, 32])
    P.inp("cst_tokid", [128, NT], I32)
    P.inp("cst_bandmask", [128, 640])
    P.inp("cst_poolB", [8, 128, 128])
    W = {}
    for l in layers:
        kind = l % 3
        if kind == 0:
            W["wqkv", l] = P.inp("wqkv%d" % l, [D, 3 * D])
            W["wo", l] = P.inp("wo%d" % l, [D, D])
            W["abias", l] = P.inp("abias%d" % l, [32, 128, 640])
        elif kind == 1:
            W["win", l] = P.inp("win%d" % l, [D, 2 * D])
            W["bin", l] = P.inp("bin%d" % l, [128, 64])
            W["wdw", l] = P.inp("wdw%d" % l, [128, 32, 31])
            W["bdw", l] = P.inp("bdw%d" % l, [128, 32])
            W["cg", l] = P.inp("cg%d" % l, [1, D])
            W["cb", l] = P.inp("cb%d" % l, [1, D])
            W["wout", l] = P.inp("wout%d" % l, [D, D])
            W["bout", l] = P.inp("bout%d" % l, [1, D])
        else:
            W["wp", l] = P.inp("wp%d" % l, [4, 1024, 1024])
            W["psc", l] = P.inp("psc%d" % l, [1, D])
        W["wq", l] = P.inp("wq%d" % l, [D, 512])
        W["wkv", l] = P.inp("wkv%d" % l, [D, 1024])
        W["wmo", l] = P.inp("wmo%d" % l, [512, D])
        W["wr", l] = P.inp("wr%d" % l, [D, 36])
        W["br", l] = P.inp("br%d" % l, [1, 36])
        W["wg", l] = P.inp("wg%d" % l, [NEXP, D, DEXP])
        W["wu", l] = P.inp("wu%d" % l, [NEXP, D, DEXP])
        W["wd", l] = P.inp("wd%d" % l, [NEXP, DEXP, D])
        W["lng", l] = P.inp("lng%d" % l, [3, D])
        W["lnb", l] = P.inp("lnb%d" % l, [3, D])
    out_h = nc.dram_tensor("out", [geo.nt_own * 128, D], F32, kind="ExternalOutput")
    X = [DramT(nc, "X%d" % i, [T, D], F32, NT) for i in range(2)]
    XB = [DramT(nc, "XB%d" % i, [T + 128, D], BF16, NT) for i in range(2)]
    XT = [DramT(nc, "XT%d" % i, [NT, 128, KC, 128], BF16, NT) for i in range(2)]
    Fd = DramT(nc, "F", [T, D], F32, NT)
    QKV = DramT(nc, "QKV", [T, 3 * D], BF16, NT)
    AT = DramT(nc, "AT", [NT, 128, KC, 128], BF16, NT)
    UT = DramT(nc, "UT", [32, 128, PADC + T], BF16, 32)
    YS = DramT(nc, "YS", [NSLOT + 128, D], F32, 1)
    SREC = DramT(nc, "SREC", [NSLOT + 128, 2], I32, 1)
    TSLOT = DramT(nc, "TSLOT", [T, 2], I32, NT)
    MKV = DramT(nc, "MKV", [128, 2048], BF16, 1)
    MKT = DramT(nc, "MKT", [128, 1024], BF16, 1)

    attn_pos = [k for k, l in enumerate(layers) if l % 3 == 0]
    last_attn = attn_pos[-1] if (attn_pos and attn_pos[-1] > 0) else None
    def proc0(k):
        if geo.nt_halo == 0:
            return geo.own0
        if last_attn is not None and k >= last_attn:
            return geo.own0
        return geo.nt_kv if last_attn is not None else geo.own0

    P.begin()
    z, zb = P.sb("z0", [128, D], BF16)
    K.op("pool", lambda e: e.memset(z[:, :], 0.0), [], [zb])
    for i in range(2):
        K.dma("sp", lambda e, i=i: e.dma_start(out=XB[i].h[T:T + 128, :], in_=z[:, :]), zb, reads=[zb], writes=XB[i].b)
        for ti in range(geo.nt_kv):
            K.dma("sp", lambda e, i=i, ti=ti: e.dma_start(out=XT[i].h[ti, :, :, :], in_=z[:, :].rearrange("p (c t) -> p c t", c=KC)),
                  zb, reads=[zb], writes=[XT[i].b[ti]])
    P.end()

    first_kv0 = proc0(0) - 4 if layers[0] % 3 == 0 else proc0(0)
    stage_prep(P, list(range(first_kv0, NT)), x_in, XB[0], XT[0])
    cur = 0
    xres = x_in
    nl = len(layers)
    for k, l in enumerate(layers):
        kind = l % 3
        tiles = list(range(proc0(k), NT))
        lng = W["lng", l]
        lnb = W["lnb", l]
        nxt = 1 - cur
        if kind == 0:
            kv0 = tiles[0] - 4
            stage_gemm(P, list(range(kv0, NT)), XT[cur], W["wqkv", l], 3 * D, QKV, BF16)
            stage_attn(P, tiles, kv0, QKV, W["abias", l], AT)
            stage_gemm(P, tiles, AT, W["wo", l], D, Fd, F32)
            stage_ln(P, tiles, Fd, xres, lng[0:1, :], lnb[0:1, :], X[nxt], XB[nxt], XT[nxt],
                     final_out=out_h if geo.mixer_only else None)
        elif kind == 1:
            stage_glu(P, tiles, XT[cur], W["win", l], W["bin", l], UT)
            stage_dwconv(P, tiles, UT, W["wdw", l], W["bdw", l], Fd)
            stage_ln(P, tiles, Fd, None, W["cg", l][0:1, :], W["cb", l][0:1, :], None, None, AT, act=AF.Silu)
            stage_gemm(P, tiles, AT, W["wout", l], D, Fd, F32)
            stage_ln(P, tiles, Fd, xres, lng[0:1, :], lnb[0:1, :], X[nxt], XB[nxt], XT[nxt], bias_ap=W["bout", l][0:1, :],
                     final_out=out_h if geo.mixer_only else None)
        else:
            stage_pool(P, tiles, XB[cur], W["wp", l], W["psc", l][0:1, :], Fd)
            stage_ln(P, tiles, Fd, xres, lng[0:1, :], lnb[0:1, :], X[nxt], XB[nxt], XT[nxt],
                     final_out=out_h if geo.mixer_only else None)
        cur = nxt
        xres = X[cur]
        nxt = 1 - cur
        if geo.mixer_only:
            continue
        stage_memkv(P, W["wkv", l], MKV, MKT)
        stage_mem(P, tiles, xres, XT[cur], W["wq", l], W["wmo", l], MKV, MKT, lng[1:2, :], lnb[1:2, :], X[nxt], XB[nxt], XT[nxt])
        cur = nxt
        xres = X[cur]
        nxt = 1 - cur
        stage_router(P, tiles, XT[cur], W["wr", l], W["br", l], SREC, TSLOT, T)
        stage_experts(P, XB[cur], SREC, W["wg", l], W["wu", l], W["wd", l], YS)
        last = (k == nl - 1)
        stage_combine(P, tiles, xres, YS, TSLOT, lng[2:3, :], lnb[2:3, :], X[nxt], XB[nxt], XT[nxt],
                      final_out=out_h if last else None)
        cur = nxt
        xres = X[cur]
    with nc.Block() as block:
        K.emit(block)
    P.root.close()
    return P


def host_consts(geo):
    NT = geo.nt_all
    c = {}
    c["cst_ident"] = np.eye(128, dtype=np.float32)
    tp = np.arange(128)
    c["cst_tri"] = (tp[:, None] < tp[None, :]).astype(np.float32)
    c["cst_iota"] = np.tile((np.arange(32, dtype=np.float32) * CAP)[None, :], (128, 1))
    c["cst_tokid"] = (np.arange(NT, dtype=np.int32)[None, :] * 128 + np.arange(128, dtype=np.int32)[:, None]).astype(np.int32)
    q = np.arange(128)[:, None]
    j = np.arange(640)[None, :]
    bm = np.zeros((128, 640), np.float32)
    bm[(q < 64) & (j >= 576)] = NEG
    bm[(q >= 64) & (j < 64)] = NEG
    c["cst_bandmask"] = bm
    wins = (2, 4, 8, 16)
    B = np.zeros((8, 128, 128), np.float32)
    t1 = np.arange(128)[:, None]
    t = np.arange(128)[None, :]
    for g, w in enumerate(wins):
        B[g * 2] = ((t1 <= t) & (t1 > t - w)).astype(np.float32) / w - (t1 == t).astype(np.float32)
        B[g * 2 + 1] = ((t1 - 128) > (t - w)).astype(np.float32) / w
    c["cst_poolB"] = B
    return c


def host_poolB0(core, consts):
    if core != 0:
        return consts["cst_poolB"]
    wins = (2, 4, 8, 16)
    B = np.zeros((8, 128, 128), np.float32)
    t1 = np.arange(128)[:, None]
    t = np.arange(128)[None, :]
    for g, w in enumerate(wins):
        cnt = np.minimum(t + 1, w).astype(np.float32)
        B[g * 2] = ((t1 <= t) & (t1 > t - w)).astype(np.float32) / cnt - (t1 == t).astype(np.float32)
    return B


def host_layer_inputs(l, prm):
    kind, j = l % 3, l // 3
    d = {}
    if kind == 0:
        d["wqkv%d" % l] = prm["attn_w_qkv"][j]
        d["wo%d" % l] = prm["attn_w_o"][j]
        q = np.arange(128)[:, None]
        jj = np.arange(640)[None, :]
        idx = np.clip(512 + q - jj, -128, 128) + 128
        d["abias%d" % l] = np.ascontiguousarray(prm["attn_rel_bias"][j][:, idx])
    elif kind == 1:
        d["win%d" % l] = prm["conv_w_in"][j]
        d["bin%d" % l] = np.ascontiguousarray(prm["conv_b_in"][j].reshape(64, 128).T)
        d["wdw%d" % l] = np.ascontiguousarray(prm["conv_w_dw"][j].reshape(31, 32, 128).transpose(2, 1, 0))
        d["bdw%d" % l] = np.ascontiguousarray(prm["conv_b_dw"][j].reshape(32, 128).T)
        d["cg%d" % l] = prm["conv_ln_g"][j].reshape(1, D)
        d["cb%d" % l] = prm["conv_ln_b"][j].reshape(1, D)
        d["wout%d" % l] = prm["conv_w_out"][j]
        d["bout%d" % l] = prm["conv_b_out"][j].reshape(1, D)
    else:
        d["wp%d" % l] = prm["pool_w"][j]
        d["psc%d" % l] = prm["pool_scale"][j].reshape(1, D)
    d["wq%d" % l] = prm["mem_w_q"][l]
    d["wkv%d" % l] = prm["mem_w_kv"][l]
    d["wmo%d" % l] = prm["mem_w_o"][l]
    d["wr%d" % l] = np.ascontiguousarray(np.concatenate(
        [prm["moe_w_group"][l], prm["moe_w_router"][l].transpose(1, 0, 2).reshape(D, 32)], axis=1))
    d["br%d" % l] = np.concatenate([prm["moe_b_group"][l], prm["moe_b_router"][l].reshape(32)]).reshape(1, 36).astype(np.float32)
    d["wg%d" % l] = prm["moe_w_gate"][l]
    d["wu%d" % l] = prm["moe_w_up"][l]
    d["wd%d" % l] = prm["moe_w_down"][l]
    d["lng%d" % l] = prm["ln_g"][l]
    d["lnb%d" % l] = prm["ln_b"][l]
    return d


def host_core_inputs(geo, core, x2d, shared):
    NT = geo.nt_all
    T = NT * 128
    own = geo.nt_own * 128
    start = core * own - geo.own0 * 128
    pos = start + np.arange(T)
    valid = pos >= 0
    xe = np.zeros((T, D), np.float32)
    xe[valid] = x2d[pos[valid]]
    d = dict(shared)
    d["x_ext"] = xe
    d["kmask"] = np.where(valid, 0.0, NEG).astype(np.float32).reshape(1, T)
    d["tokvalid"] = valid.astype(np.float32).reshape(1, T)
    d["poolB0"] = host_poolB0(core, shared)
    return d


_CACHE = {}


def run_geo(geo, x2d, mem2d, prm):
    key = (geo.n_cores, geo.nt_kv, geo.nt_halo, geo.nt_own, geo.layers, geo.mixer_only)
    if key not in _CACHE:
        _CACHE[key] = build(geo)
    P = _CACHE[key]
    shared = host_consts(geo)
    shared["mem"] = np.ascontiguousarray(mem2d, dtype=np.float32)
    for l in geo.layers:
        shared.update(host_layer_inputs(l, prm))
    in_maps = [host_core_inputs(geo, c, x2d, shared) for c in range(geo.n_cores)]
    res = run_bass_kernel_spmd(P.nc, in_maps, core_ids=list(range(geo.n_cores)))
    return np.concatenate([r["out"] for r in res.results], axis=0)


def kernel(**inputs):
    prm = {k: np.asarray(v) for k, v in inputs.items()}
    x = prm.pop("x")
    mem = prm.pop("mem")
    geo = Geo(n_cores=8, nt_kv=4, nt_halo=5, nt_own=16, layers=(0, 1, 2, 3))
    out = run_geo(geo, x[0], mem[0], prm)
    return out.reshape(1, out.shape[0], D).astype(np.float32)
```
